# Optimizing a Trainium2 kernel written in Bass

```python
import jax
import jax.numpy as jnp
from jax import lax
import numpy as np

D_MODEL = 1024
BATCH = 16
SEQ = 2048
DEPTH = 1

HEAD_DIM = 64
RWKV_HEADS = 8
RWKV_DIM = RWKV_HEADS * HEAD_DIM
LORA_W = 64
LORA_A = 64
LORA_G = 128
RWKV_COLS = 3 * RWKV_DIM + LORA_W + LORA_A + LORA_G
ATTN_PATTERNS = ((128, 1), (512, 4), (2048, 16))
ATTN_HEADS_PER_GROUP = 4
ATTN_HEADS = ATTN_HEADS_PER_GROUP * len(ATTN_PATTERNS)
ATTN_DIM = ATTN_HEADS * HEAD_DIM
ATTN_COLS = 3 * ATTN_DIM
ATTN_OUT_DIM = ATTN_HEADS_PER_GROUP * HEAD_DIM
ROT_DIM = HEAD_DIM // 4
ROPE_THETA = 500000.0
N_BRANCH = 2
IN_COLS = RWKV_COLS + ATTN_COLS + N_BRANCH * D_MODEL
MOE_GROUPS = 4
EXPERTS_PER_GROUP = 8
N_EXPERTS = MOE_GROUPS * EXPERTS_PER_GROUP
TOP_K_INNER = 2
D_EXPERT = D_MODEL // 2
MOE_BLOCK = 128
NORM_EPS = 1e-6
GN_EPS = 64e-5

kernel_name = 'hybrid_rwkv7_dilated_attn_hier_moe'


def rmsnorm(x, g, eps=NORM_EPS):
    xf = x.astype(jnp.float32)
    y = xf * lax.rsqrt(jnp.mean(xf * xf, axis=-1, keepdims=True) + eps)
    return (y * g.astype(jnp.float32)).astype(x.dtype)


def partial_rope(t, positions):
    half = ROT_DIM // 2
    inv_freq = ROPE_THETA ** (-(jnp.arange(half, dtype=jnp.float32) * (2.0 / ROT_DIM)))
    ang = positions.astype(jnp.float32)[:, :, None] * inv_freq
    cos = jnp.cos(ang)[:, :, None, :]
    sin = jnp.sin(ang)[:, :, None, :]
    t1 = t[..., :half].astype(jnp.float32)
    t2 = t[..., half:ROT_DIM].astype(jnp.float32)
    rot = jnp.concatenate([t1 * cos - t2 * sin, t2 * cos + t1 * sin], axis=-1).astype(t.dtype)
    return jnp.concatenate([rot, t[..., ROT_DIM:]], axis=-1)


def wkv7_scan(r, w, k, v, a, b, reverse):
    B, T, H, N = r.shape
    seq = tuple(t.astype(jnp.float32).transpose(1, 0, 2, 3) for t in (r, w, k, v, a, b))

    def step(S, inp):
        r_t, w_t, k_t, v_t, a_t, b_t = inp
        sa = jnp.einsum('bhij,bhj->bhi', S, a_t)
        S = S * w_t[:, :, None, :] + sa[..., None] * b_t[:, :, None, :] + v_t[..., None] * k_t[:, :, None, :]
        return S, jnp.einsum('bhij,bhj->bhi', S, r_t)

    S0 = jnp.zeros((B, H, N, N), jnp.float32)
    _, y = lax.scan(step, S0, seq, reverse=reverse)
    return y.transpose(1, 0, 2, 3)


def rwkv7_branch(u, mu_prev, mu_next, w_decay0, w_decay_up, a_gate0, a_gate_up, g_up,
                 k_k, k_a, r_k, gn_w, gn_b):
    B, T, _ = u.shape
    H, N = RWKV_HEADS, HEAD_DIM
    u_prev = jnp.pad(u, ((0, 0), (1, 0), (0, 0)))[:, :T]
    u_next = jnp.pad(u, ((0, 0), (0, 1), (0, 0)))[:, 1:]
    u = u + mu_prev * (u_prev - u) + mu_next * (u_next - u)
    c = [int(i) for i in np.cumsum([0, RWKV_DIM, RWKV_DIM, RWKV_DIM, LORA_W, LORA_A, LORA_G])]
    r, k, v, xw, xa, xg = (u[..., c[i]:c[i + 1]] for i in range(6))

    def heads(t):
        return t.reshape(B, T, H, N)

    g = jax.nn.sigmoid(xg) @ g_up
    kk = heads(k * k_k).astype(jnp.float32)
    kk = kk / jnp.maximum(jnp.sqrt(jnp.sum(kk * kk, axis=-1, keepdims=True)), 1e-12)
    rh, vh = heads(r), heads(v)
    wkv = 0.0
    bonus = 0.0
    for d, reverse in enumerate((False, True)):
        w_log = -jax.nn.softplus(-(w_decay0[d] + jnp.tanh(xw) @ w_decay_up[d]).astype(jnp.float32)) - 0.5
        decay = jnp.exp(-jnp.exp(w_log))
        a = jax.nn.sigmoid(a_gate0[d] + xa @ a_gate_up[d])
        kd = heads(k * (1.0 + (a - 1.0) * k_a))
        ah = heads(a).astype(jnp.float32)
        wkv = wkv + wkv7_scan(rh, heads(decay), kd, vh, -kk, kk * ah, reverse)
        bonus = bonus + jnp.sum(rh * kd * r_k, axis=-1, keepdims=True) * vh
    mean = jnp.mean(wkv, axis=-1, keepdims=True)
    var = jnp.mean(jnp.square(wkv - mean), axis=-1, keepdims=True)
    y = ((wkv - mean) * lax.rsqrt(var + GN_EPS)).reshape(B, T, RWKV_DIM) * gn_w + gn_b
    y = y + bonus.reshape(B, T, RWKV_DIM).astype(jnp.float32)
    return (y * g).astype(u.dtype)


def banded_attention(q, k, v, band):
    B, H, L, Dh = q.shape
    nb = -(-L // band)
    Lp = nb * band
    qb = jnp.pad(q, ((0, 0), (0, 0), (0, Lp - L), (0, 0))).reshape(B, H, nb, band, Dh)

    def windows(t):
        tb = jnp.pad(t, ((0, 0), (0, 0), (band, Lp - L + band), (0, 0))).reshape(B, H, nb + 2, band, Dh)
        return jnp.concatenate([tb[:, :, :-2], tb[:, :, 1:-1], tb[:, :, 2:]], axis=3)

    kw, vw = windows(k), windows(v)
    s = jnp.einsum('bhnqd,bhnkd->bhnqk', qb, kw, preferred_element_type=jnp.float32) * (Dh ** -0.5)
    qpos = jnp.arange(nb)[:, None, None] * band + jnp.arange(band)[None, :, None]
    kpos = (jnp.arange(nb)[:, None, None] - 1) * band + jnp.arange(3 * band)[None, None, :]
    mask = (jnp.abs(kpos - qpos) <= band) & (kpos >= 0) & (kpos < L)
    s = jnp.where(mask, s, -jnp.inf)
    lse = jax.nn.logsumexp(s, axis=-1)
    p = jnp.exp(s - lse[..., None])
    o = jnp.einsum('bhnqk,bhnkd->bhnqd', p, vw.astype(jnp.float32))
    o = o.reshape(B, H, Lp, Dh)[:, :, :L].astype(v.dtype)
    return o, lse.reshape(B, H, Lp)[:, :, :L]


def dilated_window_attention(q, k, v, dilation, band):
    B, H, T, Dh = q.shape
    L = T // dilation

    def to_sub(t):
        return t.reshape(B, H, L, dilation, Dh).transpose(0, 1, 3, 2, 4).reshape(B, H * dilation, L, Dh)

    o, lse = banded_attention(to_sub(q), to_sub(k), to_sub(v), band)
    o = o.reshape(B, H, dilation, L, Dh).transpose(0, 1, 3, 2, 4).reshape(B, H, T, Dh)
    lse = lse.reshape(B, H, dilation, L).transpose(0, 1, 3, 2).reshape(B, H, T)
    return o, lse


def dilated_attention_branch(u, positions, q_norm_g, k_norm_g):
    B, T, _ = u.shape
    q = u[..., :ATTN_DIM].reshape(B, T, ATTN_HEADS, HEAD_DIM)
    k = u[..., ATTN_DIM:2 * ATTN_DIM].reshape(B, T, ATTN_HEADS, HEAD_DIM)
    v = u[..., 2 * ATTN_DIM:].reshape(B, T, ATTN_HEADS, HEAD_DIM)
    q = partial_rope(rmsnorm(q, q_norm_g), positions)
    k = partial_rope(rmsnorm(k, k_norm_g), positions)
    q, k, v = (t.transpose(0, 2, 1, 3) for t in (q, k, v))
    outs, lses = [], []
    for gi, (window, dilation) in enumerate(ATTN_PATTERNS):
        hs = slice(gi * ATTN_HEADS_PER_GROUP, (gi + 1) * ATTN_HEADS_PER_GROUP)
        o, l = dilated_window_attention(q[:, hs], k[:, hs], v[:, hs], dilation, window // (2 * dilation))
        outs.append(o)
        lses.append(l)
    alpha = jax.nn.softmax(jnp.stack(lses), axis=0)
    o = jnp.sum(alpha[..., None] * jnp.stack(outs).astype(jnp.float32), axis=0)
    return o.transpose(0, 2, 1, 3).reshape(B, T, ATTN_OUT_DIM).astype(u.dtype)


def hier_moe(h, w_router_group, b_router_group, w_router_expert, b_router_expert, w_gate, w_up, w_down):
    N, D = h.shape
    tok_idx = jnp.arange(N, dtype=jnp.int32)
    grp_logits = (h @ w_router_group).astype(jnp.float32) + b_router_group.astype(jnp.float32)
    grp_probs = jax.nn.softmax(grp_logits, axis=-1)
    grp = jnp.argmax(grp_logits, axis=-1).astype(jnp.int32)
    grp_p = grp_probs[tok_idx, grp][:, None]
    exp_logits = ((h @ w_router_expert).astype(jnp.float32)
                  + b_router_expert.astype(jnp.float32)).reshape(N, MOE_GROUPS, EXPERTS_PER_GROUP)
    exp_logits = exp_logits[tok_idx, grp]
    top_v, top_i = lax.top_k(exp_logits, TOP_K_INNER)
    gate = grp_p * jax.nn.softmax(top_v, axis=-1)
    eid = (grp[:, None] * EXPERTS_PER_GROUP + top_i).reshape(-1)
    tok = jnp.repeat(tok_idx, TOP_K_INNER)
    wts = gate.reshape(-1)
    A = N * TOP_K_INNER
    order = jnp.argsort(eid)
    e_sorted = eid[order]
    counts = jnp.bincount(eid, length=N_EXPERTS)
    padded = (counts + MOE_BLOCK - 1) // MOE_BLOCK * MOE_BLOCK
    pad_end = jnp.cumsum(padded)
    pad_start = pad_end - padded
    start = jnp.cumsum(counts) - counts
    dest = pad_start[e_sorted] + jnp.arange(A, dtype=jnp.int32) - start[e_sorted]
    M = (-(-A // MOE_BLOCK) + N_EXPERTS) * MOE_BLOCK
    n_blk = M // MOE_BLOCK
    row_tok = jnp.full((M,), N, jnp.int32).at[dest].set(tok[order])
    row_w = jnp.zeros((M,), jnp.float32).at[dest].set(wts[order])
    blk_e = jnp.minimum(jnp.searchsorted(pad_end, jnp.arange(n_blk, dtype=jnp.int32) * MOE_BLOCK, side='right'),
                        N_EXPERTS - 1)
    h_pad = jnp.concatenate([h, jnp.zeros((1, D), h.dtype)], axis=0)
    xb = h_pad[row_tok].reshape(n_blk, MOE_BLOCK, D)

    def expert_block(args):
        xblk, e = args
        return (jax.nn.silu(xblk @ w_gate[e]) * (xblk @ w_up[e])) @ w_down[e]

    yb = lax.map(expert_block, (xb, blk_e)).reshape(M, D)
    y = jnp.zeros((N + 1, D), h.dtype).at[row_tok].add(yb * row_w[:, None].astype(h.dtype))
    return y[:N]


def hybrid_layer(x, positions, norm1_g, w_in, mu_prev, mu_next, w_decay0, w_decay_up, a_gate0, a_gate_up,
                 g_up, k_k, k_a, r_k, gn_w, gn_b, w_rwkv_out, q_norm_g, k_norm_g, w_attn_out, b_merge, w_out,
                 norm2_g, w_router_group, b_router_group, w_router_expert, b_router_expert, w_gate, w_up, w_down):
    B, T, D = x.shape
    h = rmsnorm(x, norm1_g)
    proj = h @ w_in
    u_rwkv = proj[..., :RWKV_COLS]
    u_attn = proj[..., RWKV_COLS:RWKV_COLS + ATTN_COLS]
    gate_logits = proj[..., RWKV_COLS + ATTN_COLS:].reshape(B, T, N_BRANCH, D) + b_merge
    y_rwkv = rwkv7_branch(u_rwkv, mu_prev, mu_next, w_decay0, w_decay_up, a_gate0, a_gate_up, g_up,
                          k_k, k_a, r_k, gn_w, gn_b) @ w_rwkv_out
    y_attn = dilated_attention_branch(u_attn, positions, q_norm_g, k_norm_g) @ w_attn_out
    gates = jax.nn.sigmoid(gate_logits)
    x = x + (gates[:, :, 0] * y_rwkv + gates[:, :, 1] * y_attn) @ w_out
    h2 = rmsnorm(x, norm2_g)
    moe = hier_moe(h2.reshape(B * T, D), w_router_group, b_router_group, w_router_expert, b_router_expert,
                   w_gate, w_up, w_down)
    return x + moe.reshape(B, T, D)


def setup_inputs(seed: int = 0) -> dict:
    key = jax.random.key(seed)
    ks = iter(jax.random.split(key, 32))

    def nrm(shape, scale):
        return scale * jax.random.normal(next(ks), shape, jnp.float32)

    def unif(shape, lo, hi):
        return jax.random.uniform(next(ks), shape, dtype=jnp.float32, minval=lo, maxval=hi)

    L = DEPTH
    x = nrm((BATCH, SEQ, D_MODEL), 1.0)
    positions = (jnp.arange(SEQ, dtype=jnp.int32)[None, :]
                 + jax.random.randint(next(ks), (BATCH, 1), 0, 1024, dtype=jnp.int32))
    return {
        'x': x,
        'positions': positions,
        'norm1_g': 1.0 + nrm((L, D_MODEL), 0.02),
        'w_in': nrm((L, D_MODEL, IN_COLS), D_MODEL ** -0.5),
        'mu_prev': unif((L, RWKV_COLS), 0.0, 0.5),
        'mu_next': unif((L, RWKV_COLS), 0.0, 0.5),
        'w_decay0': unif((L, 2, RWKV_DIM), -5.0, -1.0),
        'w_decay_up': nrm((L, 2, LORA_W, RWKV_DIM), 0.5 * LORA_W ** -0.5),
        'a_gate0': nrm((L, 2, RWKV_DIM), 0.1),
        'a_gate_up': nrm((L, 2, LORA_A, RWKV_DIM), LORA_A ** -0.5),
        'g_up': nrm((L, LORA_G, RWKV_DIM), LORA_G ** -0.5),
        'k_k': 0.85 + nrm((L, RWKV_DIM), 0.05),
        'k_a': 1.0 + nrm((L, RWKV_DIM), 0.05),
        'r_k': nrm((L, RWKV_HEADS, HEAD_DIM), 0.1),
        'gn_w': 1.0 + nrm((L, RWKV_DIM), 0.02),
        'gn_b': nrm((L, RWKV_DIM), 0.02),
        'w_rwkv_out': nrm((L, RWKV_DIM, D_MODEL), RWKV_DIM ** -0.5),
        'q_norm_g': 1.0 + nrm((L, HEAD_DIM), 0.02),
        'k_norm_g': 1.0 + nrm((L, HEAD_DIM), 0.02),
        'w_attn_out': nrm((L, ATTN_OUT_DIM, D_MODEL), ATTN_OUT_DIM ** -0.5),
        'b_merge': nrm((L, N_BRANCH, D_MODEL), 0.02),
        'w_out': nrm((L, D_MODEL, D_MODEL), D_MODEL ** -0.5),
        'norm2_g': 1.0 + nrm((L, D_MODEL), 0.02),
        'w_router_group': nrm((L, D_MODEL, MOE_GROUPS), D_MODEL ** -0.5),
        'b_router_group': nrm((L, MOE_GROUPS), 0.01),
        'w_router_expert': nrm((L, D_MODEL, N_EXPERTS), D_MODEL ** -0.5),
        'b_router_expert': nrm((L, N_EXPERTS), 0.01),
        'w_gate': nrm((L, N_EXPERTS, D_MODEL, D_EXPERT), D_MODEL ** -0.5),
        'w_up': nrm((L, N_EXPERTS, D_MODEL, D_EXPERT), D_MODEL ** -0.5),
        'w_down': nrm((L, N_EXPERTS, D_EXPERT, D_MODEL), D_EXPERT ** -0.5),
    }


def reference(x, positions, norm1_g, w_in, mu_prev, mu_next, w_decay0, w_decay_up, a_gate0, a_gate_up,
              g_up, k_k, k_a, r_k, gn_w, gn_b, w_rwkv_out, q_norm_g, k_norm_g, w_attn_out, b_merge, w_out,
              norm2_g, w_router_group, b_router_group, w_router_expert, b_router_expert, w_gate, w_up, w_down):
    for l in range(DEPTH):
        x = hybrid_layer(x, positions, norm1_g[l], w_in[l], mu_prev[l], mu_next[l], w_decay0[l], w_decay_up[l],
                         a_gate0[l], a_gate_up[l], g_up[l], k_k[l], k_a[l], r_k[l], gn_w[l], gn_b[l],
                         w_rwkv_out[l], q_norm_g[l], k_norm_g[l], w_attn_out[l], b_merge[l], w_out[l],
                         norm2_g[l], w_router_group[l], b_router_group[l], w_router_expert[l],
                         b_router_expert[l], w_gate[l], w_up[l], w_down[l])
    return x
```

```python
import numpy as np
from contextlib import ExitStack
import concourse.bass as bass
import concourse.mybir as mybir
from concourse.bass_utils import run_bass_kernel_spmd

F32 = mybir.dt.float32
I32 = mybir.dt.int32
U32 = mybir.dt.uint32
AF = mybir.ActivationFunctionType
ALU = mybir.AluOpType
AX = mybir.AxisListType

SAME_ENGINE_SYNC = True
TRACE = [False]
EXTRA = [False]


class Dep:
    def __init__(self):
        self.w = None
        self.r = {}


class Sem:
    def __init__(self, h, name):
        self.h = h
        self.name = name


class Tile(Dep):
    def __init__(self, t):
        Dep.__init__(self)
        self.t = t
        self.dsem = None
        self.dcount = 0
        self.ddep = Dep()

    def __getitem__(self, idx):
        return self.t[idx]

    def ap(self):
        return self.t[:]


class Prog:
    def __init__(self, nc, es):
        self.nc = nc
        self.es = es
        self.engs = {'tensor': nc.tensor, 'vector': nc.vector, 'scalar': nc.scalar,
                     'gpsimd': nc.gpsimd, 'sync': nc.sync}
        self.esem = {}
        self.ecnt = {}
        for e in ('tensor', 'vector', 'scalar', 'gpsimd'):
            self.esem[e] = Sem(es.enter_context(nc.semaphore("sem_" + e)), e)
            self.ecnt[e] = 0
        self.waited = {e: {} for e in self.engs}
        self.dma_tiles = []
        self.keep = []
        self.oneshot = []
        self.nsem = 4
        self.nops = 0
        self.epoch = 0

    def tile(self, name, shape, dtype=F32, es=None):
        t = (es or self.es).enter_context(self.nc.sbuf_tensor(name, list(shape), dtype))
        return Tile(t)

    def psum(self, name, shape=(128, 512), dtype=F32, es=None):
        t = (es or self.es).enter_context(self.nc.psum_tensor(name, list(shape), dtype))
        return Tile(t)

    def _collect(self, eng, reads, writes):
        waits = {}

        def add(ev):
            if ev is None or ev[2] != self.epoch:
                return
            s, v, _ = ev
            k = id(s)
            if k not in waits or waits[k][1] < v:
                waits[k] = (s, v)
        for d in reads:
            add(d.w)
        for d in writes:
            add(d.w)
            for ev in d.r.values():
                add(ev)
        out = []
        wd = self.waited[eng]
        for k, (s, v) in waits.items():
            if eng in self.esem and s is self.esem[eng]:
                if eng == 'tensor' or not SAME_ENGINE_SYNC:
                    continue
            if wd.get(k, -1) >= v:
                continue
            wd[k] = v
            out.append((s, v))
        return out

    def _commit(self, ev, reads, writes):
        k = id(ev[0])
        for d in reads:
            d.r[k] = ev
        for d in writes:
            d.w = ev
            d.r = {}

    def op(self, eng, fn, reads=(), writes=()):
        e = self.engs[eng]
        ws = self._collect(eng, reads, writes)
        for s, v in ws:
            e.wait_ge(s.h, v)
        if TRACE[0]:
            print("OP", eng, self.ecnt[eng] + 1, [(s.name, v) for s, v in ws])
        inst = fn(e)
        self.ecnt[eng] += 1
        inst.then_inc(self.esem[eng].h, 1)
        self._commit((self.esem[eng], self.ecnt[eng], self.epoch), reads, writes)
        self.nops += 1

    def dma(self, queue, pairs, reads=(), writes=(), tile=None, indirect=None):
        if tile is None:
            for d in list(writes) + list(reads):
                if isinstance(d, Tile):
                    tile = d
                    break
        assert tile is not None
        if indirect is not None:
            return self._dma_oneshot(queue, pairs, reads, writes, tile, indirect)
        if tile.dsem is None:
            tile.dsem = Sem(self.nc.alloc_semaphore(name="dsem%d" % self.nsem), "dma%d" % self.nsem)
            tile.dcount = 0
            self.keep.append(tile.dsem)
            self.nsem += 1
            self.dma_tiles.append(tile)
        e = self.engs[queue]
        ws = self._collect(queue, list(reads), list(writes) + [tile.ddep])
        for s, v in ws:
            e.wait_ge(s.h, v)
        for (o, i) in pairs:
            if indirect is not None:
                inst = e.indirect_dma_start(out=o, in_=i, **indirect)
            else:
                inst = e.dma_start(out=o, in_=i)
            inst.then_inc(tile.dsem.h, 16)
            tile.dcount += 16
        ev = (tile.dsem, tile.dcount, self.epoch)
        self._commit(ev, reads, list(writes) + [tile.ddep])
        self.nops += 1

    def _dma_oneshot(self, queue, pairs, reads, writes, tile, indirect):
        if len(self.oneshot) >= 40:
            self.end_phase()
        sem = Sem(self.nc.alloc_semaphore(name="osem%d" % self.nsem), "one%d" % self.nsem)
        self.nsem += 1
        self.keep.append(sem)
        e = self.engs[queue]
        ws = self._collect(queue, list(reads), list(writes) + [tile.ddep])
        for s, v in ws:
            e.wait_ge(s.h, v)
        n = 0
        for (o, i) in pairs:
            e.indirect_dma_start(out=o, in_=i, **indirect).then_inc(sem.h, 16)
            n += 16
        self.oneshot.append((sem, n))
        self._commit((sem, n, self.epoch), reads, list(writes) + [tile.ddep])
        self.nops += 1

    def barrier(self):
        evs = [(self.esem[e], self.ecnt[e]) for e in self.esem if self.ecnt[e] > 0]
        evs += [(t.dsem, t.dcount) for t in self.dma_tiles if t.dcount > 0]
        evs += list(self.oneshot)
        for eng, e in self.engs.items():
            wd = self.waited[eng]
            for s, v in evs:
                if wd.get(id(s), -1) >= v:
                    continue
                wd[id(s)] = v
                e.wait_ge(s.h, v)

    def end_phase(self):
        self.barrier()
        self.nc.all_engine_barrier()
        sems = [t.dsem.h for t in self.dma_tiles] + [s_.h for s_, _ in self.oneshot]
        self.oneshot = []
        if sems:
            self.nc.clear_and_free_semaphores(sems)
        self.nc.all_engine_barrier()
        for t in self.dma_tiles:
            t.dsem = None
            t.dcount = 0
        self.dma_tiles = []
        self.epoch += 1

    def finish(self):
        self.barrier()

    def mm(self, ps, out, lhsT, rhs, start=True, stop=True, reads=()):
        self.op('tensor', lambda e: e.matmul(out, lhsT, rhs, start=start, stop=stop),
                reads=reads, writes=[ps])

    def transpose(self, ps, out, in_, ident, reads=()):
        self.op('tensor', lambda e: e.transpose(out, in_, ident[0:in_.shape[0], 0:in_.shape[0]]),
                reads=list(reads) + [ident], writes=[ps])

    def copy(self, eng, out, in_, reads=(), writes=()):
        if eng == 'scalar':
            self.op('scalar', lambda e: e.copy(out, in_), reads=reads, writes=writes)
        else:
            self.op(eng, lambda e: e.tensor_copy(out, in_), reads=reads, writes=writes)

    def tt(self, eng, out, in0, in1, op, reads=(), writes=()):
        self.op(eng, lambda e: e.tensor_tensor(out=out, in0=in0, in1=in1, op=op), reads=reads, writes=writes)

    def ts(self, eng, out, in0, s1, s2, op0, op1=None, reads=(), writes=()):
        if op1 is None:
            self.op(eng, lambda e: e.tensor_scalar(out=out, in0=in0, scalar1=s1, scalar2=None, op0=op0),
                    reads=reads, writes=writes)
        else:
            self.op(eng, lambda e: e.tensor_scalar(out=out, in0=in0, scalar1=s1, scalar2=s2, op0=op0, op1=op1),
                    reads=reads, writes=writes)

    def act(self, out, in_, func, reads=(), writes=(), **kw):
        self.op('scalar', lambda e: e.activation(out=out, in_=in_, func=func, **kw), reads=reads, writes=writes)

    def memset(self, eng, tile, ap, val):
        self.op(eng, lambda e: e.memset(ap, val), reads=[], writes=[tile])

    def make_identity(self, ident, n=128):
        self.memset('gpsimd', ident, ident[:, :], 1.0)
        self.op('gpsimd', lambda e: e.affine_select(out=ident[:, :], in_=ident[:, :], pattern=[[1, n]],
                                                     compare_op=ALU.is_equal, fill=0.0, base=0,
                                                     channel_multiplier=-1),
                reads=[ident], writes=[ident])


CONST = {}


def rmsnorm_tile(P, xt, gb, h, junk, ss, rstd, D, eps):
    P.act(junk[:, 0:D], xt[:, 0:D], AF.Square, reads=[xt], writes=[junk, ss], accum_out=ss[:, 0:1])
    P.ts('vector', ss[:, 0:1], ss[:, 0:1], 1.0 / D, eps, ALU.mult, ALU.add, reads=[ss], writes=[ss])
    mh = CONST['mhalf']
    P.tt('gpsimd', rstd[:, 0:1], ss[:, 0:1], mh[:, 0:1], ALU.pow, reads=[ss, mh], writes=[rstd])
    P.op('vector', lambda e: e.scalar_tensor_tensor(out=h[:, 0:D], in0=xt[:, 0:D], scalar=rstd[:, 0:1],
                                                    in1=gb[:, 0:D], op0=ALU.mult, op1=ALU.mult),
         reads=[xt, rstd, gb], writes=[h])


T = 2048
NB = 2
NT = NB * T
D = 1024
INC = 6144
RC = 1792
AC = 2304
NTILE = NT // 128
NCOL = 4616
CAP = 384
NROWS = 32 * CAP + 1
TRASH = 32 * CAP
C_FA, C_FR, C_MAK, C_MRB, C_MRK, C_N, C_BH, C_KH, C_V, C_GAM = [512 * i for i in range(10)]


def bc(ap, n=128):
    return ap.broadcast_to([n, ap.shape[-1]])


def phase1(P, io, ps, ident):
    x, Pj, w_in = io['x'], io['Pj'], io['w_in']
    with ExitStack() as les:
        gb = P.tile("p1_gb", [128, D], es=les)
        P.dma('sync', [(gb.ap(), bc(io['norm1_g']))], writes=[gb])
        hT = P.tile("p1_hT", [128, 8, T], es=les)
        xt = [P.tile("p1_x%d" % i, [128, D], es=les) for i in range(2)]
        h = [P.tile("p1_h%d" % i, [128, D], es=les) for i in range(2)]
        junk = P.tile("p1_junk", [128, D], es=les)
        ss = P.tile("p1_ss", [128, 1], es=les)
        rstd = P.tile("p1_rstd", [128, 1], es=les)
        wblk = [P.tile("p1_w%d" % i, [128, 8, 512], es=les) for i in range(2)]
        ob = [P.tile("p1_o%d" % i, [128, 512], es=les) for i in range(4)]
        w_v = w_in.rearrange("(c p) n -> p c n", p=128)
        cnt = 0
        for b in range(NB):
            for i in range(16):
                n0 = b * T + i * 128
                xx, hh = xt[i % 2], h[i % 2]
                P.dma('sync', [(xx.ap(), x[n0:n0 + 128, :])], writes=[xx])
                rmsnorm_tile(P, xx, gb, hh, junk, ss, rstd, D, 1e-6)
                for half in range(2):
                    pst = ps[(2 * i + half) % 4]
                    for q in range(4):
                        c = half * 4 + q
                        P.transpose(pst, pst[:, q * 128:(q + 1) * 128], hh[:, c * 128:(c + 1) * 128], ident, reads=[hh])
                    P.copy('scalar' if half else 'vector', hT[:, half * 4:half * 4 + 4, i * 128:(i + 1) * 128],
                           pst[:, :].rearrange("p (c t) -> p c t", c=4), reads=[pst], writes=[hT])
            ncb = INC // 512
            P.dma('sync', [(wblk[0].ap(), w_v[:, :, 0:512])], writes=[wblk[0]])
            for cb in range(ncb):
                wb = wblk[cb % 2]
                if cb + 1 < ncb:
                    P.dma('sync', [(wblk[(cb + 1) % 2].ap(), w_v[:, :, (cb + 1) * 512:(cb + 2) * 512])], writes=[wblk[(cb + 1) % 2]])
                for i in range(16):
                    n0 = b * T + i * 128
                    pst = ps[4 + cnt % 4]
                    o = ob[cnt % 4]
                    for c in range(8):
                        P.mm(pst, pst[:, :], hT[:, c, i * 128:(i + 1) * 128], wb[:, c, :], start=(c == 0), stop=(c == 7),
                             reads=[hT, wb])
                    P.copy('scalar' if cnt % 2 else 'vector', o[:, :], pst[:, :], reads=[pst], writes=[o])
                    P.dma('sync', [(Pj[n0:n0 + 128, cb * 512:(cb + 1) * 512], o.ap())], reads=[o])
                    cnt += 1


class StopBuild(Exception):
    pass


STOP = [None]


CHKCNT = {}


def chk(n):
    CHKCNT[n] = CHKCNT.get(n, 0) + 1
    if STOP[0] is not None and STOP[0] % 100 == n and CHKCNT[n] == STOP[0] // 100 + 1:
        raise StopBuild()


class PsRing:
    def __init__(self, ps):
        self.ps = ps
        self.i = 0

    def next(self):
        p = self.ps[self.i % len(self.ps)]
        self.i += 1
        return p


def build_masks(P, es):
    def mk(name, w=128):
        return P.tile(name, [128, w], es=es)
    B = mk("m_B")
    P.memset('gpsimd', B, B[:, :], 1.0)
    P.op('gpsimd', lambda e: e.affine_select(out=B[:, 0:64], in_=B[:, 0:64], pattern=[[0, 64]], compare_op=ALU.is_ge,
                                             fill=0.0, base=63, channel_multiplier=-1), reads=[B], writes=[B])
    P.op('gpsimd', lambda e: e.affine_select(out=B[:, 64:128], in_=B[:, 64:128], pattern=[[0, 64]], compare_op=ALU.is_ge,
                                             fill=0.0, base=-64, channel_multiplier=1), reads=[B], writes=[B])
    res = {'B': B}
    specs = {'S0': (1, -1, ALU.is_gt), 'I0': (1, -1, ALU.is_ge), 'S1': (-1, 1, ALU.is_gt), 'I1': (-1, 1, ALU.is_ge)}
    for nm, (st, cm, cmp) in specs.items():
        Mt = mk("m_" + nm)
        P.op('gpsimd', lambda e, Mt=Mt, st=st, cm=cm, cmp=cmp: e.affine_select(
            out=Mt[:, :], in_=B[:, :], pattern=[[st, 128]], compare_op=cmp, fill=0.0, base=0, channel_multiplier=cm),
            reads=[B], writes=[Mt])
        res[nm] = Mt
    for d in range(2):
        M2 = mk("m_M2%d" % d, 256)
        P.copy('gpsimd', M2[:, 0:128], res['S%d' % d][:, :], reads=[res['S%d' % d]], writes=[M2])
        P.copy('gpsimd', M2[:, 128:256], res['I%d' % d][:, :], reads=[res['I%d' % d]], writes=[M2])
        res['M2%d' % d] = M2
    return res


def _interleave(*gens):
    gens = [g for g in gens if g is not None]
    while gens:
        for g in list(gens):
            try:
                next(g)
            except StopIteration:
                gens.remove(g)


def _interleave_w(*pairs):
    st = [[g, float(n), 0] for g, n in pairs if g is not None]
    while st:
        st.sort(key=lambda t: t[2] / t[1])
        t = st[0]
        try:
            next(t[0])
            t[2] += 1
        except StopIteration:
            st.remove(t)


def phase2(P, io, ps, ident, only=None):
    Pj, SC, GB = io['Pj'], io['SC'], io['GB']
    ring = PsRing(ps)
    with ExitStack() as les:
        def tl(name, shape=(128, 512)):
            return P.tile("p2_" + name, list(shape), es=les)
        MK = build_masks(P, les)
        mup, mun = tl("mup", (128, RC)), tl("mun", (128, RC))
        P.dma('sync', [(mup.ap(), bc(io['mu_prev']))], writes=[mup])
        P.dma('sync', [(mun.ap(), bc(io['mu_next']))], writes=[mun])
        w0b, a0b, wup, aup = [], [], [], []
        for d in range(2):
            t_ = tl("w0b%d" % d); P.dma('sync', [(t_.ap(), bc(io['w_decay0'][d:d + 1, :]))], writes=[t_]); w0b.append(t_)
            t_ = tl("a0b%d" % d); P.dma('sync', [(t_.ap(), bc(io['a_gate0'][d:d + 1, :]))], writes=[t_]); a0b.append(t_)
            t_ = tl("wup%d" % d, (64, 512)); P.dma('sync', [(t_.ap(), io['w_decay_up'][d])], writes=[t_]); wup.append(t_)
            t_ = tl("aup%d" % d, (64, 512)); P.dma('sync', [(t_.ap(), io['a_gate_up'][d])], writes=[t_]); aup.append(t_)
        gup = tl("gup"); P.dma('sync', [(gup.ap(), io['g_up'])], writes=[gup])
        kab, omka, kkb, rkb = tl("kab"), tl("omka"), tl("kkb"), tl("rkb")
        P.dma('sync', [(kab.ap(), bc(io['k_a']))], writes=[kab])
        P.dma('sync', [(kkb.ap(), bc(io['k_k']))], writes=[kkb])
        P.dma('sync', [(rkb.ap(), bc(io['r_k']))], writes=[rkb])
        P.ts('vector', omka[:, :], kab[:, :], -1.0, 1.0, ALU.mult, ALU.add, reads=[kab], writes=[omka])

        HC = RC // 2
        uh = [[tl("%s%d" % (n, k), (128, HC)) for k in range(2)] for n in ("u", "up", "un", "t1")]
        us = tl("us", (128, RC))
        xl = tl("xl", (128, 256))
        lT = tl("lT", (128, 384))
        lw = [tl("lw%d" % d) for d in range(2)]
        ad = [tl("ad%d" % d) for d in range(2)]
        kd = [tl("kd%d" % d) for d in range(2)]
        bd = [tl("bd%d" % d) for d in range(2)]
        At, Rt, Bt, Kt, BH, KH = tl("At"), tl("Rt"), tl("Bt"), tl("Kt"), tl("BH"), tl("KH")
        e_incl, e_inv, e_excl, e_rem, etot, elw = tl("e_incl"), tl("e_inv"), tl("e_excl"), tl("e_rem"), tl("etot"), tl("elw")
        tmp = [elw, etot, Kt, BH]
        gt, bon, kkt, kkn = At, Rt, Bt, tl("kkn")
        ssk, inv, bs0, bs1 = tl("ssk", (128, 8)), tl("inv", (128, 8)), tl("bs0", (128, 8)), tl("bs1", (128, 8))
        FAR = tl("FAR", (64, 8, 256))
        FBK = tl("FBK", (64, 8, 256))
        MKt = tl("MKt", (128, 8, 256))
        gam = tl("gam", (64, 16))
        MBt = [tl("MBt%d" % d, (128, 8, 256)) for d in range(2)]
        XTa = [tl("XTa%d" % d, (128, 8, 128)) for d in range(2)]
        Xa = [tl("Xa%d" % d, (128, 8, 128)) for d in range(2)]
        Xb = [tl("Xb%d" % d, (128, 8, 128)) for d in range(2)]
        XTb = [tl("XTb%d" % d, (128, 8, 128)) for d in range(2)]
        Nacc = [tl("Nacc%d" % d, (128, 8, 128)) for d in range(2)]

        def v3(ap, a):
            return ap.rearrange("p (a b) -> p a b", a=a)

        def g_prep(b, i):
            n0 = b * T + i * 128
            for k in range(2):
                cs = slice(k * HC, (k + 1) * HC)
                u, up, un, t1 = uh[0][k], uh[1][k], uh[2][k], uh[3][k]
                P.dma('sync', [(u.ap(), Pj[n0:n0 + 128, cs])], writes=[u])
                if i == 0:
                    P.memset('gpsimd', up, up[:, :], 0.0)
                    P.dma('sync', [(up[1:128, :], Pj[n0:n0 + 127, cs])], writes=[up])
                else:
                    P.dma('sync', [(up.ap(), Pj[n0 - 1:n0 + 127, cs])], writes=[up])
                if i == 15:
                    P.memset('gpsimd', un, un[:, :], 0.0)
                    P.dma('sync', [(un[0:127, :], Pj[n0 + 1:n0 + 128, cs])], writes=[un])
                else:
                    P.dma('sync', [(un.ap(), Pj[n0 + 1:n0 + 129, cs])], writes=[un])
            yield
            for k in range(2):
                cs = slice(k * HC, (k + 1) * HC)
                u, up, un, t1 = uh[0][k], uh[1][k], uh[2][k], uh[3][k]
                P.tt('gpsimd', t1[:, :], up[:, :], u[:, :], ALU.subtract, reads=[up, u], writes=[t1])
                P.tt('gpsimd', t1[:, :], t1[:, :], mup[:, cs], ALU.mult, reads=[t1, mup], writes=[t1])
                P.tt('gpsimd', up[:, :], un[:, :], u[:, :], ALU.subtract, reads=[un, u], writes=[up])
                P.tt('gpsimd', up[:, :], up[:, :], mun[:, cs], ALU.mult, reads=[up, mun], writes=[up])
                P.tt('vector', us[:, cs], u[:, :], t1[:, :], ALU.add, reads=[u, t1], writes=[us])
                P.tt('vector', us[:, cs], us[:, cs], up[:, :], ALU.add, reads=[us, up], writes=[us])
                yield
            r_, k_, v_ = us[:, 0:512], us[:, 512:1024], us[:, 1024:1536]
            P.act(xl[:, 0:64], us[:, 1536:1600], AF.Tanh, reads=[us], writes=[xl])
            P.act(xl[:, 128:256], us[:, 1664:1792], AF.Sigmoid, reads=[us], writes=[xl])
            pA = ring.next()
            P.transpose(pA, pA[0:64, 0:128], xl[:, 0:64], ident, reads=[xl])
            P.transpose(pA, pA[0:64, 128:256], us[:, 1600:1664], ident, reads=[us])
            P.transpose(pA, pA[:, 256:384], xl[:, 128:256], ident, reads=[xl])
            P.copy('vector', lT[0:64, 0:256], pA[0:64, 0:256], reads=[pA], writes=[lT])
            P.copy('vector', lT[:, 256:384], pA[:, 256:384], reads=[pA], writes=[lT])
            yield
            for d in range(2):
                pz = ring.next()
                P.mm(pz, pz[:, :], lT[0:64, 0:128], wup[d][:, :], reads=[lT, wup[d]])
                P.tt('vector', tmp[0][:, :], pz[:, :], w0b[d][:, :], ALU.add, reads=[pz, w0b[d]], writes=[tmp[0]])
                P.act(tmp[0][:, :], tmp[0][:, :], AF.Sigmoid, reads=[tmp[0]], writes=[tmp[0]])
                P.ts('vector', lw[d][:, :], tmp[0][:, :], -0.6065306597126334, None, ALU.mult, reads=[tmp[0]], writes=[lw[d]])
                pz = ring.next()
                P.mm(pz, pz[:, :], lT[0:64, 128:256], aup[d][:, :], reads=[lT, aup[d]])
                P.tt('vector', tmp[1][:, :], pz[:, :], a0b[d][:, :], ALU.add, reads=[pz, a0b[d]], writes=[tmp[1]])
                P.act(ad[d][:, :], tmp[1][:, :], AF.Sigmoid, reads=[tmp[1]], writes=[ad[d]])
                yield
            pz = ring.next()
            P.mm(pz, pz[:, :], lT[:, 256:384], gup[:, :], reads=[lT, gup])
            P.copy('scalar', gt[:, :], pz[:, :], reads=[pz], writes=[gt])
            P.dma('sync', [(GB[n0:n0 + 128, 0, :], gt.ap())], reads=[gt])
            P.tt('vector', kkt[:, :], k_, kkb[:, :], ALU.mult, reads=[us, kkb], writes=[kkt])
            P.tt('gpsimd', tmp[2][:, :], kkt[:, :], kkt[:, :], ALU.mult, reads=[kkt], writes=[tmp[2]])
            P.op('vector', lambda e: e.tensor_reduce(out=ssk[:, :], in_=v3(tmp[2][:, :], 8), axis=AX.X, op=ALU.add),
                 reads=[tmp[2]], writes=[ssk])
            mh = CONST['mhalf']
            P.tt('gpsimd', inv[:, :], ssk[:, :], mh[:, 0:8], ALU.pow, reads=[ssk, mh], writes=[inv])
            P.ts('vector', inv[:, :], inv[:, :], 1e12, None, ALU.min, reads=[inv], writes=[inv])
            P.tt('vector', v3(kkn[:, :], 8), v3(kkt[:, :], 8), inv[:, :].unsqueeze(2).to_broadcast([128, 8, 64]), ALU.mult,
                 reads=[kkt, inv], writes=[kkn])
            yield
            bs = [bs0, bs1]
            for d in range(2):
                P.tt('gpsimd', tmp[3][:, :], ad[d][:, :], kab[:, :], ALU.mult, reads=[ad[d], kab], writes=[tmp[3]])
                P.tt('gpsimd', tmp[3][:, :], tmp[3][:, :], omka[:, :], ALU.add, reads=[tmp[3], omka], writes=[tmp[3]])
                P.tt('vector', kd[d][:, :], k_, tmp[3][:, :], ALU.mult, reads=[us, tmp[3]], writes=[kd[d]])
                P.tt('gpsimd', bd[d][:, :], kkn[:, :], ad[d][:, :], ALU.mult, reads=[kkn, ad[d]], writes=[bd[d]])
                P.tt('vector', tmp[2][:, :], r_, kd[d][:, :], ALU.mult, reads=[us, kd[d]], writes=[tmp[2]])
                P.tt('vector', tmp[2][:, :], tmp[2][:, :], rkb[:, :], ALU.mult, reads=[tmp[2], rkb], writes=[tmp[2]])
                P.op('vector', lambda e, d=d: e.tensor_reduce(out=bs[d][:, :], in_=v3(tmp[2][:, :], 8), axis=AX.X, op=ALU.add),
                     reads=[tmp[2]], writes=[bs[d]])
                yield
            P.tt('vector', bs0[:, :], bs0[:, :], bs1[:, :], ALU.add, reads=[bs0, bs1], writes=[bs0])
            P.tt('vector', v3(bon[:, :], 8), v3(v_, 8), bs0[:, :].unsqueeze(2).to_broadcast([128, 8, 64]), ALU.mult,
                 reads=[us, bs0], writes=[bon])
            P.dma('sync', [(GB[n0:n0 + 128, 1, :], bon.ap())], reads=[bon])
            yield

        def g_early(b, i, d):
            r0 = i * 128
            SCd = SC[b, d]
            r_, v_ = us[:, 0:512], us[:, 1024:1536]
            MB = MBt[d]
            P.dma('sync', [(SCd[r0:r0 + 128, C_V:C_V + 512], v_)], reads=[us], tile=us)
            pc, pt = ring.next(), ring.next()
            P.mm(pc, pc[:, :], MK['I%d' % d][:, :], lw[d][:, :], reads=[MK['I%d' % d], lw[d]])
            P.mm(pt, pt[:, :], MK['B'][:, :], lw[d][:, :], reads=[MK['B'], lw[d]])
            P.act(e_incl[:, :], pc[:, :], AF.Exp, reads=[pc], writes=[e_incl])
            P.act(e_inv[:, :], pc[:, :], AF.Exp, reads=[pc], writes=[e_inv], scale=-1.0)
            P.act(elw[:, :], lw[d][:, :], AF.Exp, reads=[lw[d]], writes=[elw], scale=-1.0)
            P.act(etot[:, :], pt[:, :], AF.Exp, reads=[pt], writes=[etot])
            yield
            P.tt('gpsimd', e_excl[:, :], e_incl[:, :], elw[:, :], ALU.mult, reads=[e_incl, elw], writes=[e_excl])
            P.tt('gpsimd', e_rem[:, :], etot[:, :], e_inv[:, :], ALU.mult, reads=[etot, e_inv], writes=[e_rem])
            P.tt('vector', Rt[:, :], r_, e_incl[:, :], ALU.mult, reads=[us, e_incl], writes=[Rt])
            P.tt('vector', Kt[:, :], kd[d][:, :], e_inv[:, :], ALU.mult, reads=[kd[d], e_inv], writes=[Kt])
            P.tt('gpsimd', Bt[:, :], bd[d][:, :], e_inv[:, :], ALU.mult, reads=[bd[d], e_inv], writes=[Bt])
            P.op('vector', lambda e: e.scalar_tensor_tensor(out=At[:, :], in0=kkn[:, :], scalar=-1.0, in1=e_excl[:, :],
                                                            op0=ALU.mult, op1=ALU.mult),
                 reads=[kkn, e_excl], writes=[At])
            yield
            P.tt('gpsimd', BH[:, :], bd[d][:, :], e_rem[:, :], ALU.mult, reads=[bd[d], e_rem], writes=[BH])
            P.tt('gpsimd', KH[:, :], kd[d][:, :], e_rem[:, :], ALU.mult, reads=[kd[d], e_rem], writes=[KH])
            P.dma('sync', [(SCd[r0:r0 + 128, C_BH:C_BH + 512], BH.ap())], reads=[BH])
            P.dma('sync', [(SCd[r0:r0 + 128, C_KH:C_KH + 512], KH.ap())], reads=[KH])
            pg = ring.next()
            for hh in range(8):
                P.mm(pg, pg[0:64, hh * 2:hh * 2 + 2], lw[d][:, hh * 64:(hh + 1) * 64], MK['B'][:, 0:128:64],
                     reads=[lw[d], MK['B']])
            P.act(gam[:, :].rearrange("p (c h) -> p h c", c=2), pg[0:64, 0:16].rearrange("p (h c) -> p h c", c=2), AF.Exp,
                  reads=[pg], writes=[gam])
            for half in range(2):
                rr = r0 + half * 64
                P.dma('sync', [(SCd[rr:rr + 64, C_GAM:C_GAM + 8], gam[:, half * 8:half * 8 + 8])], reads=[gam])
            yield
            for (src, dst, off) in ((At, FAR, 0), (Rt, FAR, 128), (Bt, FBK, 0), (Kt, FBK, 128)):
                for hg in range(2):
                    pq = ring.next()
                    for q in range(4):
                        hh = hg * 4 + q
                        P.transpose(pq, pq[0:64, q * 128:(q + 1) * 128], src[:, hh * 64:(hh + 1) * 64], ident, reads=[src])
                    P.copy('scalar' if hg else 'vector', dst[:, hg * 4:hg * 4 + 4, off:off + 128],
                           v3(pq[0:64, :], 4), reads=[pq], writes=[dst])
                yield
            for half in range(2):
                rr = r0 + half * 64
                P.dma('sync', [(v3(SCd[rr:rr + 64, C_FA:C_FA + 512], 8), FAR[:, :, half * 64:half * 64 + 64]),
                               (v3(SCd[rr:rr + 64, C_FR:C_FR + 512], 8), FAR[:, :, 128 + half * 64:192 + half * 64])],
                      reads=[FAR])
            for (off, Mt_) in ((128, MKt), (0, MB)):
                for hp in range(4):
                    pq = ring.next()
                    for q in range(2):
                        hh = hp * 2 + q
                        P.mm(pq, pq[:, q * 256:(q + 1) * 256], FBK[:, hh, off:off + 128], FAR[:, hh, :], reads=[FBK, FAR])
                    P.tt('vector', Mt_[:, hp * 2:hp * 2 + 2, :], v3(pq[:, :], 2),
                         MK['M2%d' % d][:, :].unsqueeze(1).to_broadcast([128, 2, 256]), ALU.mult,
                         reads=[pq, MK['M2%d' % d]], writes=[Mt_])
                    if hp % 2:
                        yield
            for hg in range(2):
                pq = ring.next()
                for q in range(4):
                    hh = hg * 4 + q
                    P.mm(pq, pq[:, q * 128:(q + 1) * 128], FAR[:, hh, 0:128], FBK[:, hh, 0:128], reads=[FBK, FAR])
                P.tt('vector', XTa[d][:, hg * 4:hg * 4 + 4, :], v3(pq[:, :], 4),
                     MK['S%d' % (1 - d)][:, :].unsqueeze(1).to_broadcast([128, 4, 128]), ALU.mult,
                     reads=[pq, MK['S%d' % (1 - d)]], writes=[XTa[d]])
            yield
            for half in range(2):
                rr = r0 + half * 64
                hs = slice(half * 64, half * 64 + 64)
                P.dma('sync', [(v3(SCd[rr:rr + 64, C_MAK:C_MAK + 512], 8), MKt[hs, :, half * 64:half * 64 + 64]),
                               (v3(SCd[rr:rr + 64, C_MRK:C_MRK + 512], 8), MKt[hs, :, 128 + half * 64:192 + half * 64])],
                      reads=[MKt])
                P.dma('sync', [(v3(SCd[rr:rr + 64, C_MRB:C_MRB + 512], 8), MB[hs, :, 128 + half * 64:192 + half * 64])],
                      reads=[MB])
            yield

        def g_inv(b, i, d):
            r0 = i * 128
            SCd = SC[b, d]
            MB, Na = MBt[d], Nacc[d]
            P.tt('vector', Na[:, :, :], MB[:, :, 0:128], ident[:, :].unsqueeze(1).to_broadcast([128, 8, 128]), ALU.add,
                 reads=[MB, ident], writes=[Na])
            X_ap, X_t = (lambda hh: MB[:, hh, 0:128]), MB
            XT_t = XTa[d]
            outs = [(Xa[d], XTb[d]), (Xb[d], XTa[d]), (Xa[d], XTb[d]), (Xb[d], XTa[d]), (Xa[d], XTb[d])]
            for kq in range(5):
                Xn_t, XTn_t = outs[kq]
                for hg in range(2):
                    if kq < 4:
                        pq = ring.next()
                        for q in range(4):
                            hh = hg * 4 + q
                            P.mm(pq, pq[:, q * 128:(q + 1) * 128], XT_t[:, hh, :], X_ap(hh), reads=[XT_t, X_t])
                        P.copy('scalar', Xn_t[:, hg * 4:hg * 4 + 4, :], v3(pq[:, :], 4), reads=[pq], writes=[Xn_t])
                    pq = ring.next()
                    for q in range(4):
                        hh = hg * 4 + q
                        P.mm(pq, pq[:, q * 128:(q + 1) * 128], X_ap(hh), XT_t[:, hh, :], reads=[XT_t, X_t])
                    P.copy('scalar', XTn_t[:, hg * 4:hg * 4 + 4, :], v3(pq[:, :], 4), reads=[pq], writes=[XTn_t])
                    yield
                for hg in range(2):
                    pq = ring.next()
                    for q in range(4):
                        hh = hg * 4 + q
                        P.mm(pq, pq[:, q * 128:(q + 1) * 128], XTn_t[:, hh, :], Na[:, hh, :], reads=[XTn_t, Na])
                    P.tt('vector', Na[:, hg * 4:hg * 4 + 4, :], v3(pq[:, :], 4), Na[:, hg * 4:hg * 4 + 4, :], ALU.add,
                         reads=[pq, Na], writes=[Na])
                    yield
                X_t = Xn_t
                X_ap = (lambda hh, X_t=X_t: X_t[:, hh, :])
                XT_t = XTn_t
            for half in range(2):
                rr = r0 + half * 64
                hs = slice(half * 64, half * 64 + 64)
                P.dma('sync', [(v3(SCd[rr:rr + 64, C_N:C_N + 512], 8), Na[hs, :, half * 64:half * 64 + 64])], reads=[Na])
            yield

        def chain(*gs):
            for g in gs:
                yield from g
        tiles = [(b, i) for b in range(NB) for i in range(16) if only is None or (b, i) in only]
        if tiles:
            _interleave(chain(g_prep(*tiles[0]), g_early(*tiles[0], 0)))
        for ti, (b, i) in enumerate(tiles):
            _interleave(g_early(b, i, 1), g_inv(b, i, 0))
            nxt = tiles[ti + 1] if ti + 1 < len(tiles) else None
            _interleave(chain(g_prep(*nxt), g_early(*nxt, 0)) if nxt else None, g_inv(b, i, 1))


def phase3_gen(P, io, ps, les, nsteps=32):
    SC, YD = io['SC'], io['YD']

    def tl(name, shape=(128, 512)):
        return P.tile("p3_" + name, list(shape), es=les)
    G = {}
    for b in range(NB):
        G[b] = dict(rec=[tl("rec%d_%d" % (b, k), (128, NCOL)) for k in range(2)], S=tl("S%d" % b),
                    XT=tl("XT%d" % b), UT=tl("UT%d" % b), Y=[tl("Y%d_%d" % (b, k)) for k in range(2)],
                    pA=[ps[4 * b], ps[4 * b + 1]], pB=[ps[4 * b + 2], ps[4 * b + 3]])
        P.memset('vector', G[b]['S'], G[b]['S'][:, :], 0.0)

    def load(b, s):
        R = G[b]['rec'][s % 2]
        pairs = []
        for d in range(2):
            c = s if d == 0 else 31 - s
            pairs.append((R[d * 64:(d + 1) * 64, :], SC[b, d, c * 64:(c + 1) * 64, :]))
        P.dma('sync', pairs, writes=[R])
    for b in range(NB):
        load(b, 0)
    for s in range(nsteps):
        for b in range(NB):
            g = G[b]
            if s + 1 < nsteps:
                load(b, s + 1)
            R, S, XT, UT = g['rec'][s % 2], g['S'], g['XT'], g['UT']
            Y = g['Y'][s % 2]
            for d in range(2):
                p0 = d * 64
                pr = slice(p0, p0 + 64)
                pA, pB = g['pA'][d], g['pB'][d]

                def hs(base, h):
                    return R[pr, base + h * 64:base + (h + 1) * 64]

                def sl(t_, h):
                    return t_[pr, h * 64:(h + 1) * 64]
                for h in range(8):
                    P.mm(pA, sl(pA, h), hs(C_FA, h), sl(S, h), start=True, stop=False, reads=[R, S])
                    P.mm(pA, sl(pA, h), hs(C_MAK, h), hs(C_V, h), start=False, stop=True, reads=[R])
                P.copy('scalar', XT[pr, :], pA[pr, :], reads=[pA], writes=[XT])
            for d in range(2):
                p0 = d * 64
                pr = slice(p0, p0 + 64)
                pA, pB = g['pA'][d], g['pB'][d]

                def hs(base, h):
                    return R[pr, base + h * 64:base + (h + 1) * 64]

                def sl(t_, h):
                    return t_[pr, h * 64:(h + 1) * 64]
                for h in range(8):
                    P.mm(pB, sl(pB, h), hs(C_N, h), sl(XT, h), reads=[R, XT])
                P.copy('vector', UT[pr, :], pB[pr, :], reads=[pB], writes=[UT])
            for d in range(2):
                p0 = d * 64
                pr = slice(p0, p0 + 64)
                pA, pB = g['pA'][d], g['pB'][d]
                c = s if d == 0 else 31 - s

                def hs(base, h):
                    return R[pr, base + h * 64:base + (h + 1) * 64]

                def sl(t_, h):
                    return t_[pr, h * 64:(h + 1) * 64]
                for h in range(8):
                    P.mm(pA, sl(pA, h), hs(C_FR, h), sl(S, h), start=True, stop=False, reads=[R, S])
                    P.mm(pA, sl(pA, h), hs(C_MRB, h), sl(UT, h), start=False, stop=False, reads=[R, UT])
                    P.mm(pA, sl(pA, h), hs(C_MRK, h), hs(C_V, h), start=False, stop=True, reads=[R])
                for h in range(8):
                    P.mm(pB, sl(pB, h), hs(C_BH, h), sl(UT, h), start=True, stop=False, reads=[R, UT])
                    P.mm(pB, sl(pB, h), hs(C_KH, h), hs(C_V, h), start=False, stop=True, reads=[R])
                P.copy('scalar', Y[pr, :], pA[pr, :], reads=[pA], writes=[Y])
                n0 = b * T + c * 64
                P.dma('sync', [(YD[d, n0:n0 + 64, :], Y[pr, :])], reads=[Y])
                P.tt('vector', S[pr, :].rearrange("p (h i) -> p h i", h=8), S[pr, :].rearrange("p (h i) -> p h i", h=8),
                     R[pr, C_GAM:C_GAM + 8].unsqueeze(2).to_broadcast([64, 8, 64]), ALU.mult, reads=[S, R], writes=[S])
                P.tt('vector', S[pr, :], pB[pr, :], S[pr, :], ALU.add, reads=[pB, S], writes=[S])
            yield


def phase3(P, io, ps, nsteps=32):
    with ExitStack() as les:
        _interleave(phase3_gen(P, io, ps, les, nsteps))


def phase4(P, io, ps, ident, only=None):
    with ExitStack() as les:
        _interleave(phase4_gen(P, io, ps, ident, les, only))


def phase4_gen(P, io, ps, ident, les, only=None):
    YD, GB, Pj, MR = io['YD'], io['GB'], io['Pj'], io['MR']
    ring = PsRing(ps)
    if True:
        def tl(name, shape=(128, 512)):
            return P.tile("p4_" + name, list(shape), es=les)
        gnw, gnb = tl("gnw"), tl("gnb")
        P.dma('sync', [(gnw.ap(), bc(io['gn_w']))], writes=[gnw])
        P.dma('sync', [(gnb.ap(), bc(io['gn_b']))], writes=[gnb])
        wro = tl("wro", (128, 4, D))
        P.dma('sync', [(wro.ap(), io['w_rwkv_out'].rearrange("(c p) n -> p c n", p=128))], writes=[wro])
        y0, y1, gb2 = tl("y0"), tl("y1"), tl("gb2", (128, 2, 512))
        xc, sq, yr = tl("xc"), tl("sq"), tl("yr")
        s1, s2 = tl("s1", (128, 8)), tl("s2", (128, 8))
        yrT = tl("yrT", (128, 4, 128))
        mr = tl("mr", (128, D))

        def v3(ap, a=8):
            return ap.rearrange("p (a b) -> p a b", a=a)

        def bc8(t_):
            return t_[:, :].unsqueeze(2).to_broadcast([128, 8, 64])
        for i in range(NTILE):
            if only is not None and i not in only:
                continue
            n0 = i * 128
            P.dma('sync', [(y0.ap(), YD[0, n0:n0 + 128, :])], writes=[y0])
            P.dma('sync', [(y1.ap(), YD[1, n0:n0 + 128, :])], writes=[y1])
            P.dma('sync', [(gb2.ap(), GB[n0:n0 + 128, :, :])], writes=[gb2])
            P.tt('vector', y0[:, :], y0[:, :], y1[:, :], ALU.add, reads=[y0, y1], writes=[y0])
            P.op('vector', lambda e: e.tensor_reduce(out=s1[:, :], in_=v3(y0[:, :]), axis=AX.X, op=ALU.add), reads=[y0], writes=[s1])
            P.ts('vector', s1[:, :], s1[:, :], 1.0 / 64, None, ALU.mult, reads=[s1], writes=[s1])
            P.tt('vector', v3(xc[:, :]), v3(y0[:, :]), bc8(s1), ALU.subtract, reads=[y0, s1], writes=[xc])
            yield
            P.tt('gpsimd', sq[:, :], xc[:, :], xc[:, :], ALU.mult, reads=[xc], writes=[sq])
            P.op('vector', lambda e: e.tensor_reduce(out=s2[:, :], in_=v3(sq[:, :]), axis=AX.X, op=ALU.add), reads=[sq], writes=[s2])
            P.ts('vector', s2[:, :], s2[:, :], 1.0 / 64, 64e-5, ALU.mult, ALU.add, reads=[s2], writes=[s2])
            mh = CONST['mhalf']
            P.tt('gpsimd', s2[:, :], s2[:, :], mh[:, 0:8], ALU.pow, reads=[s2, mh], writes=[s2])
            yield
            P.tt('vector', v3(xc[:, :]), v3(xc[:, :]), bc8(s2), ALU.mult, reads=[xc, s2], writes=[xc])
            P.tt('gpsimd', xc[:, :], xc[:, :], gnw[:, :], ALU.mult, reads=[xc, gnw], writes=[xc])
            P.tt('gpsimd', xc[:, :], xc[:, :], gnb[:, :], ALU.add, reads=[xc, gnb], writes=[xc])
            yield
            P.tt('vector', xc[:, :], xc[:, :], gb2[:, 1, :], ALU.add, reads=[xc, gb2], writes=[xc])
            P.tt('vector', yr[:, :], xc[:, :], gb2[:, 0, :], ALU.mult, reads=[xc, gb2], writes=[yr])
            yield
            if 'YR' in io:
                P.dma('sync', [(io['YR'][n0:n0 + 128, :], yr.ap())], reads=[yr])
            pq = ring.next()
            for c in range(4):
                P.transpose(pq, pq[:, c * 128:(c + 1) * 128], yr[:, c * 128:(c + 1) * 128], ident, reads=[yr])
            P.copy('scalar', yrT[:, :, :], pq[:, :].rearrange("p (c t) -> p c t", c=4), reads=[pq], writes=[yrT])
            yield
            for hf in range(2):
                pq = ring.next()
                for c in range(4):
                    P.mm(pq, pq[:, :], yrT[:, c, :], wro[:, c, hf * 512:(hf + 1) * 512], start=(c == 0), stop=(c == 3), reads=[yrT, wro])
                P.copy('vector' if hf else 'scalar', mr[:, hf * 512:(hf + 1) * 512], pq[:, :], reads=[pq], writes=[mr])
                yield
            P.dma('sync', [(MR[n0:n0 + 128, :], mr.ap())], reads=[mr])
            yield


IN_SPECS = [
    ('x', [NT, D], F32), ('pos', [NT, 1], I32), ('norm1_g', [1, D], F32), ('w_in', [D, INC], F32),
    ('mu_prev', [1, RC], F32), ('mu_next', [1, RC], F32), ('w_decay0', [2, 512], F32),
    ('w_decay_up', [2, 64, 512], F32), ('a_gate0', [2, 512], F32), ('a_gate_up', [2, 64, 512], F32),
    ('g_up', [128, 512], F32), ('k_k', [1, 512], F32), ('k_a', [1, 512], F32), ('r_k', [1, 512], F32),
    ('gn_w', [1, 512], F32), ('gn_b', [1, 512], F32), ('w_rwkv_out', [512, D], F32),
    ('q_norm_g', [1, 64], F32), ('k_norm_g', [1, 64], F32), ('w_attn_out', [256, D], F32),
    ('b_merge', [2, D], F32), ('w_out', [D, D], F32), ('norm2_g', [1, D], F32),
    ('w_router', [D, 36], F32), ('b_router', [1, 36], F32),
    ('w_gate', [32 * D, 512], F32), ('w_up', [32 * D, 512], F32), ('w_down', [32 * 512, D], F32),
]
SCRATCH = [
    ('Pj', [NT, INC]), ('SC', [NB, 2, T, NCOL]), ('GB', [NT, 2, 512]), ('YD', [2, NT, 512]), ('MR', [NT, D]),
    ('QKV', [NT, AC]), ('OA', [3, NT, 260]), ('X1', [NT, D]), ('XE', [NROWS, D]), ('YE', [NROWS, D]),
]


def build_program(phases, dbg_out=(), dbg_in=(), opts=None):
    opts = opts or {}
    nc = bass.Bass("TRN2", target_bir_lowering=False)
    io = {}
    for name, shape, dt in IN_SPECS:
        if opts.get('inputs') is not None and name not in opts['inputs']:
            continue
        io[name] = nc.dram_tensor(name, list(shape), dt, kind="ExternalInput").ap()
    for name, shape in SCRATCH + list(opts.get('extra_scratch', [])):
        kind = "ExternalOutput" if name in dbg_out else ("ExternalInput" if name in dbg_in else "Internal")
        io[name] = nc.dram_tensor(name, list(shape), F32, kind=kind).ap()
        io['d_' + name] = Dep()
    io['y'] = nc.dram_tensor("y", [NT, D], F32, kind="ExternalOutput").ap()
    with ExitStack() as es:
        P = Prog(nc, es)
        ident = P.tile("ident", [128, 128])
        P.make_identity(ident)
        ps = [P.psum("ps%d" % i) for i in range(8)]
        RT = P.tile("RT", [128, NTILE, 2])
        CONST['mhalf'] = P.tile("c_mhalf", [128, 24])
        P.memset('gpsimd', CONST['mhalf'], CONST['mhalf'][:, :], -0.5)
        CONST['e'] = P.tile("c_e", [128, 4])
        P.memset('gpsimd', CONST['e'], CONST['e'][:, :], float(np.e))
        RTi = P.tile("RTi", [128, NTILE, 2], I32)
        for ph in phases:
            if ph == 1:
                phase1(P, io, ps, ident)
            elif ph == 2:
                phase2(P, io, ps, ident, only=opts.get('only2'))
            elif ph == 3:
                phase3(P, io, ps, nsteps=opts.get('nsteps', 32))
            elif ph == 35:
                with ExitStack() as les:
                    _interleave_w((phase3_gen(P, io, ps, les, opts.get('nsteps', 32)), 64), (phase5a_gen(P, io, les, opts.get('only5a')), 128))
            elif ph == 50:
                phase5b(P, io, ps, ident, only=opts.get('only5b'))
            elif ph == 45:
                with ExitStack() as les:
                    if 'XE' in io:
                        XE, YE = io['XE'], io['YE']
                        zt = P.tile("p45_zt", [128, 2, D], es=les)
                        P.memset('gpsimd', zt, zt[:, :, :], 0.0)
                        xev = XE[0:32 * CAP, :].rearrange("(a p r) n -> a p r n", p=128, r=2)
                        for a in range(32 * CAP // 256):
                            P.dma('gpsimd', [(xev[a], zt.ap())], reads=[zt])
                        P.dma('gpsimd', [(XE[TRASH:TRASH + 1, :], zt[0:1, 0, :]), (YE[TRASH:TRASH + 1, :], zt[0:1, 0, :])], reads=[zt])
                    g5 = phase5b_gen(P, io, ps[2:8], ident, les, opts.get('only5b'))
                    for _ in range(16):
                        next(g5, None)
                    _interleave(g5, phase4_gen(P, io, ps[0:2], ident, les, opts.get('only4')))
            elif ph == 4:
                phase4(P, io, ps, ident, only=opts.get('only4'))
            elif ph == 5:
                phase5a(P, io, ps, only=opts.get('only5a'))
                P.end_phase()
                phase5b(P, io, ps, ident, only=opts.get('only5b'))
            elif ph == 6:
                phase6(P, io, ps, ident, RT, RTi, only=opts.get('only6'))
            elif ph == 7:
                phase7(P, io, ps, ident, experts=opts.get('experts', range(32)))
            elif ph == 8:
                phase8(P, io, RT, RTi, only=opts.get('only6'))
            P.end_phase()
        P.finish()
    return nc


def make_inputs(inputs, core):
    b0 = core * NB
    f = np.ascontiguousarray
    m = {
        'x': f(inputs['x'][b0:b0 + NB].reshape(NT, D)),
        'pos': f(inputs['positions'][b0:b0 + NB].reshape(NT, 1).astype(np.int32)),
        'w_in': f(inputs['w_in'][0]),
        'w_decay0': f(inputs['w_decay0'][0]), 'w_decay_up': f(inputs['w_decay_up'][0]),
        'a_gate0': f(inputs['a_gate0'][0]), 'a_gate_up': f(inputs['a_gate_up'][0]),
        'g_up': f(inputs['g_up'][0]), 'w_rwkv_out': f(inputs['w_rwkv_out'][0]),
        'w_attn_out': f(inputs['w_attn_out'][0]), 'b_merge': f(inputs['b_merge'][0]), 'w_out': f(inputs['w_out'][0]),
        'w_router': f(np.concatenate([inputs['w_router_group'][0], inputs['w_router_expert'][0]], axis=1)),
        'b_router': f(np.concatenate([inputs['b_router_group'][0], inputs['b_router_expert'][0]], axis=0).reshape(1, 36)),
        'w_gate': f(inputs['w_gate'][0].reshape(32 * D, 512)), 'w_up': f(inputs['w_up'][0].reshape(32 * D, 512)),
        'w_down': f(inputs['w_down'][0].reshape(32 * 512, D)),
    }
    for k in ('norm1_g', 'mu_prev', 'mu_next', 'k_k', 'k_a', 'r_k', 'gn_w', 'gn_b', 'q_norm_g', 'k_norm_g', 'norm2_g'):
        m[k] = f(inputs[k].reshape(1, -1))
    return m


ATT_PAT = ((1, 2048), (4, 512), (16, 128))
TWO_PI_HI = 6.28125
TWO_PI_LO = 2.0 * np.pi - 6.28125
MAGIC = 12582912.0


def phase5a(P, io, ps, only=None):
    with ExitStack() as les:
        _interleave(phase5a_gen(P, io, les, only))


def phase5a_gen(P, io, les, only=None):
    Pj, QKV, pos = io['Pj'], io['QKV'], io['pos']
    if True:
        def tl(name, shape, dt=F32):
            return P.tile("p5a_" + name, list(shape), dt, es=les)
        gq, gk = tl("gq", (128, 64)), tl("gk", (128, 64))
        P.dma('sync', [(gq.ap(), bc(io['q_norm_g']))], writes=[gq])
        P.dma('sync', [(gk.ap(), bc(io['k_norm_g']))], writes=[gk])
        invf = tl("invf", (128, 8))
        for i in range(8):
            P.memset('vector', invf, invf[:, i:i + 1], float(np.float32(500000.0) ** np.float32(-(i * 2.0 / 16))))
        qkv = tl("qkv", (128, AC))
        qo = tl("qo", (128, 1536))
        sq = tl("sq", (128, 1536))
        ss = tl("ss", (128, 24))
        posi = tl("posi", (128, 1), I32)
        posf = tl("posf", (128, 1))
        ang, tA, ks_, kc_, ths, thc, sn, cs = [tl(n, (128, 8)) for n in ("ang", "tA", "ks", "kc", "ths", "thc", "sn", "cs")]
        r1, r2, tm = tl("r1", (128, 24, 8)), tl("r2", (128, 24, 8)), tl("tm", (128, 24, 8))

        def red(theta, k_, add_half_pi):
            P.op('vector', lambda e: e.scalar_tensor_tensor(out=theta[:, :], in0=k_[:, :], scalar=-TWO_PI_HI, in1=ang[:, :],
                                                            op0=ALU.mult, op1=ALU.add), reads=[k_, ang], writes=[theta])
            P.op('vector', lambda e: e.scalar_tensor_tensor(out=theta[:, :], in0=k_[:, :], scalar=-TWO_PI_LO, in1=theta[:, :],
                                                            op0=ALU.mult, op1=ALU.add), reads=[k_, theta], writes=[theta])
            if add_half_pi:
                P.ts('vector', theta[:, :], theta[:, :], float(np.pi / 2), None, ALU.add, reads=[theta], writes=[theta])
            P.ts('vector', theta[:, :], theta[:, :], float(np.pi), float(-np.pi), ALU.min, ALU.max, reads=[theta], writes=[theta])
        for i in range(NTILE):
            if only is not None and i not in only:
                continue
            n0 = i * 128
            P.dma('sync', [(qkv.ap(), Pj[n0:n0 + 128, RC:RC + AC])], writes=[qkv])
            P.dma('sync', [(posi.ap(), pos[n0:n0 + 128, :])], writes=[posi])
            P.copy('vector', posf[:, :], posi[:, :], reads=[posi], writes=[posf])
            P.ts('vector', ang[:, :], invf[:, :], posf[:, 0:1], None, ALU.mult, reads=[invf, posf], writes=[ang])
            P.ts('vector', tA[:, :], ang[:, :], float(1.0 / (2.0 * np.pi)), None, ALU.mult, reads=[ang], writes=[tA])
            P.ts('vector', ks_[:, :], tA[:, :], MAGIC, None, ALU.add, reads=[tA], writes=[ks_])
            P.ts('vector', ks_[:, :], ks_[:, :], -MAGIC, None, ALU.add, reads=[ks_], writes=[ks_])
            P.ts('vector', kc_[:, :], tA[:, :], 0.25, None, ALU.add, reads=[tA], writes=[kc_])
            P.ts('vector', kc_[:, :], kc_[:, :], MAGIC, None, ALU.add, reads=[kc_], writes=[kc_])
            P.ts('vector', kc_[:, :], kc_[:, :], -MAGIC, None, ALU.add, reads=[kc_], writes=[kc_])
            red(ths, ks_, False)
            red(thc, kc_, True)
            P.act(sn[:, :], ths[:, :], AF.Sin, reads=[ths], writes=[sn])
            P.act(cs[:, :], thc[:, :], AF.Sin, reads=[thc], writes=[cs])
            yield
            P.tt('gpsimd', sq[:, :], qkv[:, 0:1536], qkv[:, 0:1536], ALU.mult, reads=[qkv], writes=[sq])
            P.op('vector', lambda e: e.tensor_reduce(out=ss[:, :], in_=sq[:, :].rearrange("p (a b) -> p a b", a=24), axis=AX.X, op=ALU.add),
                 reads=[sq], writes=[ss])
            P.ts('vector', ss[:, :], ss[:, :], 1.0 / 64, 1e-6, ALU.mult, ALU.add, reads=[ss], writes=[ss])
            mh = CONST['mhalf']
            P.tt('gpsimd', ss[:, :], ss[:, :], mh[:, 0:24], ALU.pow, reads=[ss, mh], writes=[ss])
            yield
            P.tt('vector', qo[:, :].rearrange("p (a b) -> p a b", a=24), qkv[:, 0:1536].rearrange("p (a b) -> p a b", a=24),
                 ss[:, :].unsqueeze(2).to_broadcast([128, 24, 64]), ALU.mult, reads=[qkv, ss], writes=[qo])
            for (o, g_) in ((0, gq), (768, gk)):
                P.tt('gpsimd', qo[:, o:o + 768].rearrange("p (a b) -> p a b", a=12), qo[:, o:o + 768].rearrange("p (a b) -> p a b", a=12),
                     g_[:, :].unsqueeze(1).to_broadcast([128, 12, 64]), ALU.mult, reads=[qo, g_], writes=[qo])
            qv = qo[:, :].rearrange("p (a b) -> p a b", a=24)
            t1, t2 = qv[:, :, 0:8], qv[:, :, 8:16]
            csb = cs[:, :].unsqueeze(1).to_broadcast([128, 24, 8])
            snb = sn[:, :].unsqueeze(1).to_broadcast([128, 24, 8])
            yield
            P.tt('vector', r1[:, :, :], t1, csb, ALU.mult, reads=[qo, cs], writes=[r1])
            P.tt('vector', tm[:, :, :], t2, snb, ALU.mult, reads=[qo, sn], writes=[tm])
            P.tt('vector', r1[:, :, :], r1[:, :, :], tm[:, :, :], ALU.subtract, reads=[r1, tm], writes=[r1])
            P.tt('vector', r2[:, :, :], t2, csb, ALU.mult, reads=[qo, cs], writes=[r2])
            P.tt('vector', tm[:, :, :], t1, snb, ALU.mult, reads=[qo, sn], writes=[tm])
            P.tt('vector', r2[:, :, :], r2[:, :, :], tm[:, :, :], ALU.add, reads=[r2, tm], writes=[r2])
            P.copy('vector', t1, r1[:, :, :], reads=[r1], writes=[qo])
            P.copy('vector', t2, r2[:, :, :], reads=[r2], writes=[qo])
            P.dma('sync', [(QKV[n0:n0 + 128, 0:1536], qo.ap())], reads=[qo])
            P.dma('sync', [(QKV[n0:n0 + 128, 1536:2304], qkv[:, 1536:2304])], reads=[qkv], tile=qkv)
            yield


def phase5b(P, io, ps, ident, only=None):
    with ExitStack() as les:
        _interleave(phase5b_gen(P, io, ps, ident, les, only))


def phase5b_gen(P, io, ps, ident, les, only=None):
    QKV, OA = io['QKV'], io['OA']
    ring_o, ring_s, ring_l = PsRing(ps[0:2]), PsRing(ps[2:4]), PsRing(ps[4:6])

    def tl(name, shape, dt=F32):
        return P.tile("p5b_" + name, list(shape), dt, es=les)
    M3 = tl("M3", (128, 3, 128))
    P.memset('gpsimd', M3, M3[:, :, :], 1.0)

    def sel(ap, step, cm, base):
        P.op('gpsimd', lambda e: e.affine_select(out=ap, in_=ap, pattern=[[step, 128]], compare_op=ALU.is_ge, fill=0.0,
                                                 base=base, channel_multiplier=cm), reads=[M3], writes=[M3])
    sel(M3[:, 0, :], -1, 1, -64)
    sel(M3[:, 1, :], -1, 1, 64)
    sel(M3[:, 1, :], 1, -1, 64)
    sel(M3[:, 2, :], 1, -1, -64)
    QT = [tl("QT%d" % k, (128, 2, T)) for k in range(2)]
    KT = [tl("KT%d" % k, (128, 2, T)) for k in range(2)]
    V1 = [tl("V1_%d" % k, (128, 16, 4, 65)) for k in range(2)]
    for k in range(2):
        P.memset('vector', V1[k], V1[k][:, :, :, :], 1.0)
    Qs, Ks = [tl("Qs%d" % k, (128, 256)) for k in range(2)], [tl("Ks%d" % k, (128, 256)) for k in range(2)]
    Pt = [tl("Pt%d" % k, (128, 384)) for k in range(3)]
    Ob = [tl("Ob%d" % k, (128, 260)) for k in range(2)]
    units = [(b, g) for b in range(NB) for g in range(3) if only is None or (b, g) in only]
    cnt = [0]

    def rows(b, g, j):
        Dg, L = ATT_PAT[g]
        r, l0 = (j * 128) // L, (j * 128) % L
        st = b * T + l0 * Dg + r
        return slice(st, st + 127 * Dg + 1, Dg) if Dg > 1 else slice(st, st + 128)

    def g_load(ui):
        b, g = units[ui]
        qt_, kt_, v1 = QT[ui % 2], KT[ui % 2], V1[ui % 2]
        for j in range(16):
            q_, k_ = Qs[j % 2], Ks[j % 2]
            rs = rows(b, g, j)
            P.dma('sync', [(q_.ap(), QKV[rs, g * 256:(g + 1) * 256])], writes=[q_])
            P.dma('sync', [(k_.ap(), QKV[rs, 768 + g * 256:768 + (g + 1) * 256])], writes=[k_])
            P.dma('sync', [(v1[:, j, :, 0:64], QKV[rs, 1536 + g * 256:1536 + (g + 1) * 256].rearrange("p (h d) -> p h d", h=4))],
                  writes=[v1])
            for (src, dst) in ((q_, qt_), (k_, kt_)):
                pq = ring_l.next()
                for pr in range(2):
                    P.transpose(pq, pq[:, pr * 128:(pr + 1) * 128], src[:, pr * 128:(pr + 1) * 128], ident, reads=[src])
                P.copy('scalar' if dst is kt_ else 'vector', dst[:, :, j * 128:(j + 1) * 128],
                       pq[:, 0:256].rearrange("p (a t) -> p a t", a=2), reads=[pq], writes=[dst])
            yield

    def g_comp(ui):
        b, g = units[ui]
        Dg, L = ATT_PAT[g]
        nlt = L // 128
        qt_, kt_, v1 = QT[ui % 2], KT[ui % 2], V1[ui % 2]
        for j in range(16):
            r, qt = j // nlt, j % nlt
            kts = [kt for kt in (qt - 1, qt, qt + 1) if 0 <= kt < nlt]
            mlo = kts[0] - (qt - 1)
            nk = len(kts)
            po = ring_o.next()
            for h in range(4):
                pr, hh = h // 2, h % 2
                hp = slice(hh * 64, hh * 64 + 64)
                pS = ring_s.next()
                for ki, kt in enumerate(kts):
                    jk = r * nlt + kt
                    P.mm(pS, pS[:, ki * 128:(ki + 1) * 128], kt_[hp, pr, jk * 128:(jk + 1) * 128], qt_[hp, pr, j * 128:(j + 1) * 128],
                         reads=[kt_, qt_])
                pt_ = Pt[cnt[0] % 3]
                cnt[0] += 1
                P.act(pt_[:, 0:nk * 128], pS[:, 0:nk * 128], AF.Exp, reads=[pS], writes=[pt_], scale=0.125)
                P.tt('vector', pt_[:, 0:nk * 128].rearrange("p (a b) -> p a b", a=nk),
                     pt_[:, 0:nk * 128].rearrange("p (a b) -> p a b", a=nk), M3[:, mlo:mlo + nk, :], ALU.mult,
                     reads=[pt_, M3], writes=[pt_])
                for ki, kt in enumerate(kts):
                    jk = r * nlt + kt
                    P.mm(po, po[:, h * 65:(h + 1) * 65], pt_[:, ki * 128:(ki + 1) * 128], v1[:, jk, h, :],
                         start=(ki == 0), stop=(ki == nk - 1), reads=[pt_, v1])
                yield
            ob = Ob[j % 2]
            P.copy('scalar', ob[:, :], po[:, 0:260], reads=[po], writes=[ob])
            P.dma('sync', [(OA[g, rows(b, g, j), :], ob.ap())], reads=[ob])
    if units:
        for _ in g_load(0):
            yield
    for ui in range(len(units)):
        active = [g_comp(ui)] + ([g_load(ui + 1)] if ui + 1 < len(units) else [])
        while active:
            for gg in list(active):
                try:
                    next(gg)
                    yield
                except StopIteration:
                    active.remove(gg)


def phase6(P, io, ps, ident, RT, RTi, only=None):
    OA, MR, Pj, x, X1, XE, YE = io['OA'], io['MR'], io['Pj'], io['x'], io['X1'], io['XE'], io['YE']
    ring = PsRing(ps)
    with ExitStack() as les:
        def tl(name, shape, dt=F32):
            return P.tile("p6_" + name, list(shape), dt, es=les)
        wao = tl("wao", (128, 2, D))
        P.dma('sync', [(wao.ap(), io['w_attn_out'].rearrange("(c p) n -> p c n", p=128))], writes=[wao])
        wout = tl("wout", (128, 8, D))
        P.dma('sync', [(wout.ap(), io['w_out'].rearrange("(c p) n -> p c n", p=128))], writes=[wout])
        wr = tl("wr", (128, 8, 36))
        P.dma('sync', [(wr.ap(), io['w_router'].rearrange("(c p) n -> p c n", p=128))], writes=[wr])
        bm1, g2b, brb = tl("bm1", (128, D)), tl("g2b", (128, D)), tl("brb", (128, 36))
        bm0 = tl("bm0", (128, D))
        P.dma('sync', [(bm0.ap(), bc(io['b_merge'][0:1, :]))], writes=[bm0])
        P.dma('sync', [(bm1.ap(), bc(io['b_merge'][1:2, :]))], writes=[bm1])
        P.dma('sync', [(g2b.ap(), bc(io['norm2_g']))], writes=[g2b])
        P.dma('sync', [(brb.ap(), bc(io['b_router']))], writes=[brb])
        Us, ones = tl("Us", (128, 128)), tl("ones", (128, 128))
        P.memset('gpsimd', ones, ones[:, :], 1.0)
        P.op('gpsimd', lambda e: e.affine_select(out=Us[:, :], in_=ones[:, :], pattern=[[1, 128]], compare_op=ALU.is_gt, fill=0.0,
                                                 base=0, channel_multiplier=-1), reads=[ones], writes=[Us])
        ebi = tl("ebi", (128, 32), I32)
        P.op('gpsimd', lambda e: e.iota(ebi[:, :], pattern=[[CAP, 32]], base=-TRASH, channel_multiplier=0), reads=[], writes=[ebi])
        ebase = tl("ebase", (128, 32))
        P.copy('vector', ebase[:, :], ebi[:, :], reads=[ebi], writes=[ebase])
        Rrun = tl("Rrun", (128, 32))
        P.memset('vector', Rrun, Rrun[:, :], 0.0)

        junk = tl("junk", (128, D))

        def mkslot(j, par, base=None):
            B = {}
            sfx = "%d_%d" % (j, par)
            if base is None:
                B['oa'] = tl("oa%d" % j, (128, 3, 260))
                for n in ("mr", "gl1", "gl0", "xt", "mm"):
                    B[n] = tl("%s%d" % (n, j), (128, D))
                B['mT'] = tl("mT%d" % j, (128, 8, 128))
                B['ot'], B['oT'] = tl("ot%d" % j, (128, 256)), tl("oT%d" % j, (128, 2, 128))
                B['rden'] = tl("rden%d" % j, (128, 4))
            else:
                for n in ("oa", "mr", "gl1", "gl0", "xt", "mm", "mT", "ot", "oT", "rden"):
                    B[n] = base[n]
            B['junk'] = junk
            for n in ("x1", "h2"):
                B[n] = tl("%s%s" % (n, sfx), (128, D))
            B['h2T'] = tl("h2T" + sfx, (128, 8, 128))
            B['ss'], B['rstd'] = tl("ss" + sfx, (128, 1)), tl("rstd" + sfx, (128, 1))
            B['Lg'] = tl("Lg" + sfx, (128, 36))
            B['sm'] = {n: tl("%s_%s" % (n, sfx), (128, 1)) for n in ("gmax", "gsum", "grp_p", "m1", "m2", "dm", "e21", "den", "g1w", "g2w", "i1f", "i2f", "v1", "v2")}
            B['goh'], B['gex'] = tl("goh" + sfx, (128, 4)), tl("gex" + sfx, (128, 4))
            for n in ("selv", "A1", "A2", "Aa", "rank", "valid", "slot", "tmp32"):
                B[n] = tl("%s%s" % (n, sfx), (128, 32))
            for n in ("sel", "sel2", "oh1", "oh2"):
                B[n] = tl("%s%s" % (n, sfx), (128, 8))
            return B
        slots = {}
        for j in range(2):
            slots[(j, 0)] = mkslot(j, 0)
            slots[(j, 1)] = mkslot(j, 1, base=slots[(j, 0)])

        def v48(t_):
            return t_[:, :].rearrange("p (g e) -> p g e", g=4)

        def tr8(src, dst):
            for hf in range(2):
                pq = ring.next()
                for q in range(4):
                    c = hf * 4 + q
                    P.transpose(pq, pq[:, q * 128:(q + 1) * 128], src[:, c * 128:(c + 1) * 128], ident, reads=[src])
                P.copy('scalar' if hf else 'vector', dst[:, hf * 4:hf * 4 + 4, :], pq[:, :].rearrange("p (c t) -> p c t", c=4),
                       reads=[pq], writes=[dst])

        def st_a(i, B):
            n0 = i * 128
            oa, mr, gl1, gl0, xt, ot, oT, rden = B['oa'], B['mr'], B['gl1'], B['gl0'], B['xt'], B['ot'], B['oT'], B['rden']
            P.dma('sync', [(oa.ap(), OA[:, n0:n0 + 128, :].rearrange("g p c -> p g c"))], writes=[oa])
            P.dma('sync', [(mr.ap(), MR[n0:n0 + 128, :])], writes=[mr])
            P.dma('sync', [(gl1.ap(), Pj[n0:n0 + 128, RC + AC + D:RC + AC + 2 * D])], writes=[gl1])
            P.dma('sync', [(gl0.ap(), Pj[n0:n0 + 128, RC + AC:RC + AC + D])], writes=[gl0])
            P.dma('sync', [(xt.ap(), x[n0:n0 + 128, :])], writes=[xt])
            P.tt('vector', oa[:, 0, :], oa[:, 0, :], oa[:, 1, :], ALU.add, reads=[oa], writes=[oa])
            P.tt('vector', oa[:, 0, :], oa[:, 0, :], oa[:, 2, :], ALU.add, reads=[oa], writes=[oa])
            ov = oa[:, 0, :].rearrange("p (h c) -> p h c", h=4)
            P.op('vector', lambda e: e.reciprocal(rden[:, :].unsqueeze(2), ov[:, :, 64:65]), reads=[oa], writes=[rden])
            P.tt('vector', ot[:, :].rearrange("p (h c) -> p h c", h=4), ov[:, :, 0:64], rden[:, :].unsqueeze(2).to_broadcast([128, 4, 64]),
                 ALU.mult, reads=[oa, rden], writes=[ot])
            if 'OT' in io:
                P.dma('sync', [(io['OT'][n0:n0 + 128, :], ot.ap())], reads=[ot])
            pq = ring.next()
            for c in range(2):
                P.transpose(pq, pq[:, c * 128:(c + 1) * 128], ot[:, c * 128:(c + 1) * 128], ident, reads=[ot])
            P.copy('vector', oT[:, :, :], pq[:, 0:256].rearrange("p (c t) -> p c t", c=2), reads=[pq], writes=[oT])
            P.tt('vector', gl1[:, :], gl1[:, :], bm1[:, :], ALU.add, reads=[gl1, bm1], writes=[gl1])
            P.act(gl1[:, :], gl1[:, :], AF.Sigmoid, reads=[gl1], writes=[gl1])
            P.tt('vector', gl0[:, :], gl0[:, :], bm0[:, :], ALU.add, reads=[gl0, bm0], writes=[gl0])
            P.act(gl0[:, :], gl0[:, :], AF.Sigmoid, reads=[gl0], writes=[gl0])
            P.tt('vector', mr[:, :], mr[:, :], gl0[:, :], ALU.mult, reads=[mr, gl0], writes=[mr])

        def st_b(i, B):
            oT, gl1, mm_, mr, mT = B['oT'], B['gl1'], B['mm'], B['mr'], B['mT']
            for hf in range(2):
                pq = ring.next()
                for c in range(2):
                    P.mm(pq, pq[:, :], oT[:, c, :], wao[:, c, hf * 512:(hf + 1) * 512], start=(c == 0), stop=(c == 1), reads=[oT, wao])
                P.tt('vector', mm_[:, hf * 512:(hf + 1) * 512], pq[:, :], gl1[:, hf * 512:(hf + 1) * 512], ALU.mult,
                     reads=[pq, gl1], writes=[mm_])
            P.tt('vector', mm_[:, :], mm_[:, :], mr[:, :], ALU.add, reads=[mm_, mr], writes=[mm_])
            tr8(mm_, mT)

        def st_c(i, B):
            n0 = i * 128
            mT, xt, x1, h2, junk, ss, rstd, h2T, Lg = B['mT'], B['xt'], B['x1'], B['h2'], B['junk'], B['ss'], B['rstd'], B['h2T'], B['Lg']
            for hf in range(2):
                pq = ring.next()
                for c in range(8):
                    P.mm(pq, pq[:, :], mT[:, c, :], wout[:, c, hf * 512:(hf + 1) * 512], start=(c == 0), stop=(c == 7), reads=[mT, wout])
                P.tt('vector', x1[:, hf * 512:(hf + 1) * 512], pq[:, :], xt[:, hf * 512:(hf + 1) * 512], ALU.add,
                     reads=[pq, xt], writes=[x1])
            P.dma('sync', [(X1[n0:n0 + 128, :], x1.ap())], reads=[x1])
            rmsnorm_tile(P, x1, g2b, h2, junk, ss, rstd, D, 1e-6)
            if 'H2' in io:
                P.dma('sync', [(io['H2'][n0:n0 + 128, :], h2.ap())], reads=[h2])
            tr8(h2, h2T)
            pq = ring.next()
            for c in range(8):
                P.mm(pq, pq[:, 0:36], h2T[:, c, :], wr[:, c, :], start=(c == 0), stop=(c == 7), reads=[h2T, wr])
            P.tt('vector', Lg[:, :], pq[:, 0:36], brb[:, :], ALU.add, reads=[pq, brb], writes=[Lg])
            if 'LG' in io:
                P.dma('sync', [(io['LG'][n0:n0 + 128, :], Lg.ap())], reads=[Lg])

        def st_d(i, B):
            S_, Lg, goh, gex, selv, sel, sel2, oh1, oh2 = B['sm'], B['Lg'], B['goh'], B['gex'], B['selv'], B['sel'], B['sel2'], B['oh1'], B['oh2']
            A1, A2, Aa, rank, valid, slot, tmp32, h2 = B['A1'], B['A2'], B['Aa'], B['rank'], B['valid'], B['slot'], B['tmp32'], B['h2']
            P.op('vector', lambda e: e.tensor_reduce(out=S_['gmax'][:, :], in_=Lg[:, 0:4], axis=AX.X, op=ALU.max), reads=[Lg], writes=[S_['gmax']])
            P.ts('vector', goh[:, :], Lg[:, 0:4], S_['gmax'][:, 0:1], None, ALU.is_equal, reads=[Lg, S_['gmax']], writes=[goh])
            P.ts('vector', gex[:, :], Lg[:, 0:4], S_['gmax'][:, 0:1], None, ALU.subtract, reads=[Lg, S_['gmax']], writes=[gex])
            P.tt('gpsimd', gex[:, :], CONST['e'][:, 0:4], gex[:, :], ALU.pow, reads=[CONST['e'], gex], writes=[gex])
            P.op('vector', lambda e: e.tensor_reduce(out=S_['gsum'][:, :], in_=gex[:, :], axis=AX.X, op=ALU.add), reads=[gex], writes=[S_['gsum']])
            P.op('vector', lambda e: e.reciprocal(S_['grp_p'][:, :], S_['gsum'][:, :]), reads=[S_['gsum']], writes=[S_['grp_p']])
            P.tt('vector', v48(selv), Lg[:, 4:36].rearrange("p (g e) -> p g e", g=4), goh[:, :].unsqueeze(2).to_broadcast([128, 4, 8]),
                 ALU.mult, reads=[Lg, goh], writes=[selv])
            P.op('vector', lambda e: e.tensor_reduce(out=sel[:, :], in_=selv[:, :].rearrange("p (g e) -> p e g", g=4), axis=AX.X, op=ALU.add),
                 reads=[selv], writes=[sel])
            P.op('vector', lambda e: e.tensor_reduce(out=S_['m1'][:, :], in_=sel[:, :], axis=AX.X, op=ALU.max), reads=[sel], writes=[S_['m1']])
            P.ts('vector', oh1[:, :], sel[:, :], S_['m1'][:, 0:1], None, ALU.is_equal, reads=[sel, S_['m1']], writes=[oh1])
            P.op('vector', lambda e: e.scalar_tensor_tensor(out=sel2[:, :], in0=oh1[:, :], scalar=-1e30, in1=sel[:, :], op0=ALU.mult, op1=ALU.add),
                 reads=[oh1, sel], writes=[sel2])
            P.op('vector', lambda e: e.tensor_reduce(out=S_['m2'][:, :], in_=sel2[:, :], axis=AX.X, op=ALU.max), reads=[sel2], writes=[S_['m2']])
            P.ts('vector', oh2[:, :], sel2[:, :], S_['m2'][:, 0:1], None, ALU.is_equal, reads=[sel2, S_['m2']], writes=[oh2])
            P.tt('vector', S_['dm'][:, :], S_['m2'][:, :], S_['m1'][:, :], ALU.subtract, reads=[S_['m1'], S_['m2']], writes=[S_['dm']])
            P.tt('gpsimd', S_['e21'][:, :], CONST['e'][:, 0:1], S_['dm'][:, :], ALU.pow, reads=[CONST['e'], S_['dm']], writes=[S_['e21']])
            P.ts('vector', S_['den'][:, :], S_['e21'][:, :], 1.0, None, ALU.add, reads=[S_['e21']], writes=[S_['den']])
            P.op('vector', lambda e: e.reciprocal(S_['den'][:, :], S_['den'][:, :]), reads=[S_['den']], writes=[S_['den']])
            P.tt('vector', S_['g1w'][:, :], S_['grp_p'][:, :], S_['den'][:, :], ALU.mult, reads=[S_['grp_p'], S_['den']], writes=[S_['g1w']])
            P.tt('vector', S_['g2w'][:, :], S_['g1w'][:, :], S_['e21'][:, :], ALU.mult, reads=[S_['g1w'], S_['e21']], writes=[S_['g2w']])
            for (A_, oh_) in ((A1, oh1), (A2, oh2)):
                P.copy('vector', v48(A_), goh[:, :].unsqueeze(2).to_broadcast([128, 4, 8]), reads=[goh], writes=[A_])
                P.tt('vector', v48(A_), v48(A_), oh_[:, :].unsqueeze(1).to_broadcast([128, 4, 8]), ALU.mult, reads=[A_, oh_], writes=[A_])
            P.tt('vector', Aa[:, :], A1[:, :], A2[:, :], ALU.add, reads=[A1, A2], writes=[Aa])
            pr = ring.next()
            P.mm(pr, pr[:, 0:32], Us[:, :], Aa[:, :], reads=[Us, Aa])
            P.tt('vector', rank[:, :], pr[:, 0:32], Rrun[:, :], ALU.add, reads=[pr, Rrun], writes=[rank])
            pr2 = ring.next()
            P.mm(pr2, pr2[:, 0:32], ones[:, :], Aa[:, :], reads=[ones, Aa])
            P.tt('vector', Rrun[:, :], pr2[:, 0:32], Rrun[:, :], ALU.add, reads=[pr2, Rrun], writes=[Rrun])
            P.ts('vector', valid[:, :], rank[:, :], float(CAP), None, ALU.is_lt, reads=[rank], writes=[valid])
            P.tt('vector', slot[:, :], rank[:, :], ebase[:, :], ALU.add, reads=[rank, ebase], writes=[slot])
            P.tt('vector', slot[:, :], slot[:, :], valid[:, :], ALU.mult, reads=[slot, valid], writes=[slot])
            for (A_, if_, v_, gw) in ((A1, S_['i1f'], S_['v1'], S_['g1w']), (A2, S_['i2f'], S_['v2'], S_['g2w'])):
                P.tt('vector', tmp32[:, :], A_[:, :], slot[:, :], ALU.mult, reads=[A_, slot], writes=[tmp32])
                P.op('vector', lambda e, if_=if_: e.tensor_reduce(out=if_[:, :], in_=tmp32[:, :], axis=AX.X, op=ALU.add),
                     reads=[tmp32], writes=[if_])
                P.ts('vector', if_[:, :], if_[:, :], float(TRASH), None, ALU.add, reads=[if_], writes=[if_])
                P.tt('vector', tmp32[:, :], A_[:, :], valid[:, :], ALU.mult, reads=[A_, valid], writes=[tmp32])
                P.op('vector', lambda e, v_=v_: e.tensor_reduce(out=v_[:, :], in_=tmp32[:, :], axis=AX.X, op=ALU.add),
                     reads=[tmp32], writes=[v_])
                P.tt('vector', gw[:, :], gw[:, :], v_[:, :], ALU.mult, reads=[gw, v_], writes=[gw])
            P.copy('vector', RTi[:, i, 0:1], S_['i1f'][:, :], reads=[S_['i1f']], writes=[RTi])
            P.copy('vector', RTi[:, i, 1:2], S_['i2f'][:, :], reads=[S_['i2f']], writes=[RTi])
            P.copy('vector', RT[:, i, 0:1], S_['g1w'][:, :], reads=[S_['g1w']], writes=[RT])
            P.copy('vector', RT[:, i, 1:2], S_['g2w'][:, :], reads=[S_['g2w']], writes=[RT])

        def st_e(i, B):
            h2 = B['h2']
            for k in range(2):
                P.dma('gpsimd', [(XE[:, :], h2.ap())], reads=[h2, RTi], tile=h2,
                      indirect=dict(out_offset=bass.IndirectOffsetOnAxis(RTi[:, i, k:k + 1], 0), in_offset=None))
        tiles = [i for i in range(NTILE) if only is None or i in only]
        pairs = [tiles[g0:g0 + 2] for g0 in range(0, len(tiles), 2)]

        def run(stage, p):
            for j, i in enumerate(pairs[p]):
                stage(i, slots[(j, p % 2)])
        if pairs:
            run(st_a, 0)
            run(st_b, 0)
            run(st_c, 0)
        for p in range(len(pairs)):
            if p + 1 < len(pairs):
                run(st_a, p + 1)
                run(st_b, p + 1)
                for j in range(2):
                    if j < len(pairs[p + 1]):
                        st_c(pairs[p + 1][j], slots[(j, (p + 1) % 2)])
                    if j < len(pairs[p]):
                        st_d(pairs[p][j], slots[(j, p % 2)])
            else:
                run(st_d, p)
            run(st_e, p)


def phase7(P, io, ps, ident, experts=range(32)):
    XE, YE = io['XE'], io['YE']
    ring = PsRing(ps)
    with ExitStack() as les:
        def tl(name, shape, dt=F32):
            return P.tile("p7_" + name, list(shape), dt, es=les)
        wg = [tl("wg%d" % k, (128, 8, 512)) for k in range(2)]
        wu = [tl("wu%d" % k, (128, 8, 512)) for k in range(2)]
        wd = [tl("wd%d" % k, (128, 4, D)) for k in range(2)]
        NBUF = 2
        xb = [tl("xb%d" % k, (128, D)) for k in range(2 * NBUF)]
        xT = [tl("xT%d" % k, (128, 8, 128)) for k in range(NBUF)]
        sg = [tl("sg%d" % k, (128, 512)) for k in range(NBUF)]
        hm = [tl("hm%d" % k, (128, 512)) for k in range(NBUF)]
        hT = [tl("hT%d" % k, (128, 4, 128)) for k in range(NBUF)]
        yb = [tl("yb%d" % k, (128, D)) for k in range(NBUF)]
        wgv = io['w_gate'].rearrange("(e c p) n -> e p c n", p=128, c=8)
        wuv = io['w_up'].rearrange("(e c p) n -> e p c n", p=128, c=8)
        wdv = io['w_down'].rearrange("(e c p) n -> e p c n", p=128, c=4)
        experts = list(experts)
        nblk = CAP // 128

        def loadw(ei):
            k = ei % 2
            P.dma('sync', [(wg[k].ap(), wgv[experts[ei]])], writes=[wg[k]])
            P.dma('sync', [(wu[k].ap(), wuv[experts[ei]])], writes=[wu[k]])
            P.dma('sync', [(wd[k].ap(), wdv[experts[ei]])], writes=[wd[k]])
        blocks = [(ei, blk) for ei in range(len(experts)) for blk in range(nblk)]

        def loadx(bi):
            ei, blk = blocks[bi]
            r0 = experts[ei] * CAP + blk * 128
            P.dma('sync', [(xb[bi % (2 * NBUF)].ap(), XE[r0:r0 + 128, :])], writes=[xb[bi % (2 * NBUF)]])
        loadw(0)
        for bi in range(min(2 * NBUF, len(blocks))):
            loadx(bi)
        st = {}

        def s_tr(bi, j):
            x_ = xb[bi % (2 * NBUF)]
            for hf in range(2):
                pq = ring.next()
                for q in range(4):
                    c = hf * 4 + q
                    P.transpose(pq, pq[:, q * 128:(q + 1) * 128], x_[:, c * 128:(c + 1) * 128], ident, reads=[x_])
                P.copy('scalar' if hf else 'vector', xT[j][:, hf * 4:hf * 4 + 4, :], pq[:, :].rearrange("p (c t) -> p c t", c=4),
                       reads=[pq], writes=[xT[j]])

        def s_gu(bi, j):
            k = blocks[bi][0] % 2
            pg, pu = ring.next(), ring.next()
            for c in range(8):
                P.mm(pg, pg[:, :], xT[j][:, c, :], wg[k][:, c, :], start=(c == 0), stop=(c == 7), reads=[xT[j], wg[k]])
            for c in range(8):
                P.mm(pu, pu[:, :], xT[j][:, c, :], wu[k][:, c, :], start=(c == 0), stop=(c == 7), reads=[xT[j], wu[k]])
            P.act(sg[j][:, :], pg[:, :], AF.Silu, reads=[pg], writes=[sg[j]])
            P.tt('vector', hm[j][:, :], pu[:, :], sg[j][:, :], ALU.mult, reads=[pu, sg[j]], writes=[hm[j]])
            if bi + 2 * NBUF < len(blocks):
                loadx(bi + 2 * NBUF)

        def s_t4(bi, j):
            pq = ring.next()
            for c in range(4):
                P.transpose(pq, pq[:, c * 128:(c + 1) * 128], hm[j][:, c * 128:(c + 1) * 128], ident, reads=[hm[j]])
            P.copy('scalar', hT[j][:, :, :], pq[:, :].rearrange("p (c t) -> p c t", c=4), reads=[pq], writes=[hT[j]])

        def s_dn(bi, j):
            ei, blk = blocks[bi]
            k = ei % 2
            r0 = experts[ei] * CAP + blk * 128
            for hf in range(2):
                pq = ring.next()
                for c in range(4):
                    P.mm(pq, pq[:, :], hT[j][:, c, :], wd[k][:, c, hf * 512:(hf + 1) * 512], start=(c == 0), stop=(c == 3),
                         reads=[hT[j], wd[k]])
                P.copy('vector' if hf else 'scalar', yb[j][:, hf * 512:(hf + 1) * 512], pq[:, :], reads=[pq], writes=[yb[j]])
            P.dma('sync', [(YE[r0:r0 + 128, :], yb[j].ap())], reads=[yb[j]])
        loaded = 1
        for g0 in range(0, len(blocks), NBUF):
            grp = list(range(g0, min(g0 + NBUF, len(blocks))))
            lo, hi = min(blocks[bi][0] for bi in grp), max(blocks[bi][0] for bi in grp)
            while loaded < hi + 1:
                loadw(loaded)
                loaded += 1
            if lo == hi and loaded == hi + 1 and hi + 1 < len(experts):
                loadw(hi + 1)
                loaded += 1
            for stage in (s_tr, s_gu, s_t4, s_dn):
                for j, bi in enumerate(grp):
                    stage(bi, j)


def phase8(P, io, RT, RTi, only=None):
    X1, YE, y = io['X1'], io['YE'], io['y']
    with ExitStack() as les:
        def tl(name, shape, dt=F32):
            return P.tile("p8_" + name, list(shape), dt, es=les)
        x1 = [tl("x1_%d" % k, (128, D)) for k in range(2)]
        ya = [tl("ya_%d" % k, (128, D)) for k in range(2)]
        yb = [tl("yb_%d" % k, (128, D)) for k in range(2)]
        for i in range(NTILE):
            if only is not None and i not in only:
                continue
            n0 = i * 128
            k = i % 2
            P.dma('sync', [(x1[k].ap(), X1[n0:n0 + 128, :])], writes=[x1[k]])
            P.dma('gpsimd', [(ya[k].ap(), YE[:, :])], reads=[RTi], writes=[ya[k]],
                  indirect=dict(out_offset=None, in_offset=bass.IndirectOffsetOnAxis(RTi[:, i, 0:1], 0)))
            P.dma('gpsimd', [(yb[k].ap(), YE[:, :])], reads=[RTi], writes=[yb[k]],
                  indirect=dict(out_offset=None, in_offset=bass.IndirectOffsetOnAxis(RTi[:, i, 1:2], 0)))
            P.op('vector', lambda e: e.scalar_tensor_tensor(out=x1[k][:, :], in0=ya[k][:, :], scalar=RT[:, i, 0:1], in1=x1[k][:, :],
                                                            op0=ALU.mult, op1=ALU.add), reads=[ya[k], RT, x1[k]], writes=[x1[k]])
            P.op('vector', lambda e: e.scalar_tensor_tensor(out=x1[k][:, :], in0=yb[k][:, :], scalar=RT[:, i, 1:2], in1=x1[k][:, :],
                                                            op0=ALU.mult, op1=ALU.add), reads=[yb[k], RT, x1[k]], writes=[x1[k]])
            P.dma('sync', [(y[n0:n0 + 128, :], x1[k].ap())], reads=[x1[k]])


_NC_CACHE = {}


def kernel(**inputs):
    inputs = {k: np.asarray(v) for k, v in inputs.items()}
    if 'nc' not in _NC_CACHE:
        _NC_CACHE['nc'] = build_program([1, 2, 35, 45, 6, 7, 8])
    nc = _NC_CACHE['nc']
    n_cores = 8
    in_maps = [make_inputs(inputs, c) for c in range(n_cores)]
    res = run_bass_kernel_spmd(nc, in_maps, core_ids=list(range(n_cores)))
    out = np.stack([np.asarray(res.results[c]['y']).reshape(NB, T, D) for c in range(n_cores)], axis=0)
    return out.reshape(16, T, D).astype(np.float32)
```

```python
import numpy as np
from contextlib import ExitStack
import concourse.bass as bass
import concourse.mybir as mybir
from concourse.bass_utils import run_bass_kernel_spmd

F32 = mybir.dt.float32
I32 = mybir.dt.int32
U32 = mybir.dt.uint32
AF = mybir.ActivationFunctionType
ALU = mybir.AluOpType
AX = mybir.AxisListType

SAME_ENGINE_SYNC = True
TRACE = [False]
EXTRA = [False]


class Dep:
    def __init__(self):
        self.w = None
        self.r = {}


class Sem:
    def __init__(self, h, name):
        self.h = h
        self.name = name


class Tile(Dep):
    def __init__(self, t):
        Dep.__init__(self)
        self.t = t
        self.dsem = None
        self.dcount = 0
        self.ddep = Dep()

    def __getitem__(self, idx):
        return self.t[idx]

    def ap(self):
        return self.t[:]


class Prog:
    def __init__(self, nc, es):
        self.nc = nc
        self.es = es
        self.engs = {'tensor': nc.tensor, 'vector': nc.vector, 'scalar': nc.scalar,
                     'gpsimd': nc.gpsimd, 'sync': nc.sync}
        self.esem = {}
        self.ecnt = {}
        for e in ('tensor', 'vector', 'scalar', 'gpsimd'):
            self.esem[e] = Sem(es.enter_context(nc.semaphore("sem_" + e)), e)
            self.ecnt[e] = 0
        self.waited = {e: {} for e in self.engs}
        self.dma_tiles = []
        self.keep = []
        self.oneshot = []
        self.nsem = 4
        self.nops = 0
        self.epoch = 0

    def tile(self, name, shape, dtype=F32, es=None):
        t = (es or self.es).enter_context(self.nc.sbuf_tensor(name, list(shape), dtype))
        return Tile(t)

    def psum(self, name, shape=(128, 512), dtype=F32, es=None):
        t = (es or self.es).enter_context(self.nc.psum_tensor(name, list(shape), dtype))
        return Tile(t)

    def _collect(self, eng, reads, writes):
        waits = {}

        def add(ev):
            if ev is None or ev[2] != self.epoch:
                return
            s, v, _ = ev
            k = id(s)
            if k not in waits or waits[k][1] < v:
                waits[k] = (s, v)
        for d in reads:
            add(d.w)
        for d in writes:
            add(d.w)
            for ev in d.r.values():
                add(ev)
        out = []
        wd = self.waited[eng]
        for k, (s, v) in waits.items():
            if eng in self.esem and s is self.esem[eng]:
                if eng == 'tensor' or not SAME_ENGINE_SYNC:
                    continue
            if wd.get(k, -1) >= v:
                continue
            wd[k] = v
            out.append((s, v))
        return out

    def _commit(self, ev, reads, writes):
        k = id(ev[0])
        for d in reads:
            d.r[k] = ev
        for d in writes:
            d.w = ev
            d.r = {}

    def op(self, eng, fn, reads=(), writes=()):
        e = self.engs[eng]
        ws = self._collect(eng, reads, writes)
        for s, v in ws:
            e.wait_ge(s.h, v)
        if TRACE[0]:
            print("OP", eng, self.ecnt[eng] + 1, [(s.name, v) for s, v in ws])
        inst = fn(e)
        self.ecnt[eng] += 1
        inst.then_inc(self.esem[eng].h, 1)
        self._commit((self.esem[eng], self.ecnt[eng], self.epoch), reads, writes)
        self.nops += 1

    def dma(self, queue, pairs, reads=(), writes=(), tile=None, indirect=None):
        if tile is None:
            for d in list(writes) + list(reads):
                if isinstance(d, Tile):
                    tile = d
                    break
        assert tile is not None
        if indirect is not None:
            return self._dma_oneshot(queue, pairs, reads, writes, tile, indirect)
        if tile.dsem is None:
            tile.dsem = Sem(self.nc.alloc_semaphore(name="dsem%d" % self.nsem), "dma%d" % self.nsem)
            tile.dcount = 0
            self.keep.append(tile.dsem)
            self.nsem += 1
            self.dma_tiles.append(tile)
        e = self.engs[queue]
        ws = self._collect(queue, list(reads), list(writes) + [tile.ddep])
        for s, v in ws:
            e.wait_ge(s.h, v)
        for (o, i) in pairs:
            if indirect is not None:
                inst = e.indirect_dma_start(out=o, in_=i, **indirect)
            else:
                inst = e.dma_start(out=o, in_=i)
            inst.then_inc(tile.dsem.h, 16)
            tile.dcount += 16
        ev = (tile.dsem, tile.dcount, self.epoch)
        self._commit(ev, reads, list(writes) + [tile.ddep])
        self.nops += 1

    def _dma_oneshot(self, queue, pairs, reads, writes, tile, indirect):
        if len(self.oneshot) >= 40:
            self.end_phase()
        sem = Sem(self.nc.alloc_semaphore(name="osem%d" % self.nsem), "one%d" % self.nsem)
        self.nsem += 1
        self.keep.append(sem)
        e = self.engs[queue]
        ws = self._collect(queue, list(reads), list(writes) + [tile.ddep])
        for s, v in ws:
            e.wait_ge(s.h, v)
        n = 0
        for (o, i) in pairs:
            e.indirect_dma_start(out=o, in_=i, **indirect).then_inc(sem.h, 16)
            n += 16
        self.oneshot.append((sem, n))
        self._commit((sem, n, self.epoch), reads, list(writes) + [tile.ddep])
        self.nops += 1

    def barrier(self):
        evs = [(self.esem[e], self.ecnt[e]) for e in self.esem if self.ecnt[e] > 0]
        evs += [(t.dsem, t.dcount) for t in self.dma_tiles if t.dcount > 0]
        evs += list(self.oneshot)
        for eng, e in self.engs.items():
            wd = self.waited[eng]
            for s, v in evs:
                if wd.get(id(s), -1) >= v:
                    continue
                wd[id(s)] = v
                e.wait_ge(s.h, v)

    def end_phase(self):
        self.barrier()
        self.nc.all_engine_barrier()
        sems = [t.dsem.h for t in self.dma_tiles] + [s_.h for s_, _ in self.oneshot]
        self.oneshot = []
        if sems:
            self.nc.clear_and_free_semaphores(sems)
        self.nc.all_engine_barrier()
        for t in self.dma_tiles:
            t.dsem = None
            t.dcount = 0
        self.dma_tiles = []
        self.epoch += 1

    def finish(self):
        self.barrier()

    def mm(self, ps, out, lhsT, rhs, start=True, stop=True, reads=()):
        self.op('tensor', lambda e: e.matmul(out, lhsT, rhs, start=start, stop=stop),
                reads=reads, writes=[ps])

    def transpose(self, ps, out, in_, ident, reads=()):
        self.op('tensor', lambda e: e.transpose(out, in_, ident[0:in_.shape[0], 0:in_.shape[0]]),
                reads=list(reads) + [ident], writes=[ps])

    def copy(self, eng, out, in_, reads=(), writes=()):
        if eng == 'scalar':
            self.op('scalar', lambda e: e.copy(out, in_), reads=reads, writes=writes)
        else:
            self.op(eng, lambda e: e.tensor_copy(out, in_), reads=reads, writes=writes)

    def tt(self, eng, out, in0, in1, op, reads=(), writes=()):
        self.op(eng, lambda e: e.tensor_tensor(out=out, in0=in0, in1=in1, op=op), reads=reads, writes=writes)

    def ts(self, eng, out, in0, s1, s2, op0, op1=None, reads=(), writes=()):
        if op1 is None:
            self.op(eng, lambda e: e.tensor_scalar(out=out, in0=in0, scalar1=s1, scalar2=None, op0=op0),
                    reads=reads, writes=writes)
        else:
            self.op(eng, lambda e: e.tensor_scalar(out=out, in0=in0, scalar1=s1, scalar2=s2, op0=op0, op1=op1),
                    reads=reads, writes=writes)

    def act(self, out, in_, func, reads=(), writes=(), **kw):
        self.op('scalar', lambda e: e.activation(out=out, in_=in_, func=func, **kw), reads=reads, writes=writes)

    def memset(self, eng, tile, ap, val):
        self.op(eng, lambda e: e.memset(ap, val), reads=[], writes=[tile])

    def make_identity(self, ident, n=128):
        self.memset('gpsimd', ident, ident[:, :], 1.0)
        self.op('gpsimd', lambda e: e.affine_select(out=ident[:, :], in_=ident[:, :], pattern=[[1, n]],
                                                     compare_op=ALU.is_equal, fill=0.0, base=0,
                                                     channel_multiplier=-1),
                reads=[ident], writes=[ident])


CONST = {}


def rmsnorm_tile(P, xt, gb, h, junk, ss, rstd, D, eps):
    P.act(junk[:, 0:D], xt[:, 0:D], AF.Square, reads=[xt], writes=[junk, ss], accum_out=ss[:, 0:1])
    P.ts('vector', ss[:, 0:1], ss[:, 0:1], 1.0 / D, eps, ALU.mult, ALU.add, reads=[ss], writes=[ss])
    mh = CONST['mhalf']
    P.tt('gpsimd', rstd[:, 0:1], ss[:, 0:1], mh[:, 0:1], ALU.pow, reads=[ss, mh], writes=[rstd])
    P.op('vector', lambda e: e.scalar_tensor_tensor(out=h[:, 0:D], in0=xt[:, 0:D], scalar=rstd[:, 0:1],
                                                    in1=gb[:, 0:D], op0=ALU.mult, op1=ALU.mult),
         reads=[xt, rstd, gb], writes=[h])


T = 2048
NB = 2
NT = NB * T
D = 1024
INC = 6144
RC = 1792
AC = 2304
NTILE = NT // 128
NCOL = 4616
CAP = 384
NROWS = 32 * CAP + 1
TRASH = 32 * CAP
C_FA, C_FR, C_MAK, C_MRB, C_MRK, C_N, C_BH, C_KH, C_V, C_GAM = [512 * i for i in range(10)]


def bc(ap, n=128):
    return ap.broadcast_to([n, ap.shape[-1]])


def phase1(P, io, ps, ident):
    x, Pj, w_in = io['x'], io['Pj'], io['w_in']
    with ExitStack() as les:
        gb = P.tile("p1_gb", [128, D], es=les)
        P.dma('sync', [(gb.ap(), bc(io['norm1_g']))], writes=[gb])
        hT = P.tile("p1_hT", [128, 8, T], es=les)
        xt = [P.tile("p1_x%d" % i, [128, D], es=les) for i in range(2)]
        h = [P.tile("p1_h%d" % i, [128, D], es=les) for i in range(2)]
        junk = P.tile("p1_junk", [128, D], es=les)
        ss = P.tile("p1_ss", [128, 1], es=les)
        rstd = P.tile("p1_rstd", [128, 1], es=les)
        wblk = [P.tile("p1_w%d" % i, [128, 8, 512], es=les) for i in range(2)]
        ob = [P.tile("p1_o%d" % i, [128, 512], es=les) for i in range(4)]
        w_v = w_in.rearrange("(c p) n -> p c n", p=128)
        cnt = 0
        for b in range(NB):
            for i in range(16):
                n0 = b * T + i * 128
                xx, hh = xt[i % 2], h[i % 2]
                P.dma('sync', [(xx.ap(), x[n0:n0 + 128, :])], writes=[xx])
                rmsnorm_tile(P, xx, gb, hh, junk, ss, rstd, D, 1e-6)
                for half in range(2):
                    pst = ps[(2 * i + half) % 4]
                    for q in range(4):
                        c = half * 4 + q
                        P.transpose(pst, pst[:, q * 128:(q + 1) * 128], hh[:, c * 128:(c + 1) * 128], ident, reads=[hh])
                    P.copy('scalar' if half else 'vector', hT[:, half * 4:half * 4 + 4, i * 128:(i + 1) * 128],
                           pst[:, :].rearrange("p (c t) -> p c t", c=4), reads=[pst], writes=[hT])
            ncb = INC // 512
            P.dma('sync', [(wblk[0].ap(), w_v[:, :, 0:512])], writes=[wblk[0]])
            for cb in range(ncb):
                wb = wblk[cb % 2]
                if cb + 1 < ncb:
                    P.dma('sync', [(wblk[(cb + 1) % 2].ap(), w_v[:, :, (cb + 1) * 512:(cb + 2) * 512])], writes=[wblk[(cb + 1) % 2]])
                for i in range(16):
                    n0 = b * T + i * 128
                    pst = ps[4 + cnt % 4]
                    o = ob[cnt % 4]
                    for c in range(8):
                        P.mm(pst, pst[:, :], hT[:, c, i * 128:(i + 1) * 128], wb[:, c, :], start=(c == 0), stop=(c == 7),
                             reads=[hT, wb])
                    P.copy('scalar' if cnt % 2 else 'vector', o[:, :], pst[:, :], reads=[pst], writes=[o])
                    P.dma('sync', [(Pj[n0:n0 + 128, cb * 512:(cb + 1) * 512], o.ap())], reads=[o])
                    cnt += 1


class StopBuild(Exception):
    pass


STOP = [None]


CHKCNT = {}


def chk(n):
    CHKCNT[n] = CHKCNT.get(n, 0) + 1
    if STOP[0] is not None and STOP[0] % 100 == n and CHKCNT[n] == STOP[0] // 100 + 1:
        raise StopBuild()


class PsRing:
    def __init__(self, ps):
        self.ps = ps
        self.i = 0

    def next(self):
        p = self.ps[self.i % len(self.ps)]
        self.i += 1
        return p


def build_masks(P, es):
    def mk(name, w=128):
        return P.tile(name, [128, w], es=es)
    B = mk("m_B")
    P.memset('gpsimd', B, B[:, :], 1.0)
    P.op('gpsimd', lambda e: e.affine_select(out=B[:, 0:64], in_=B[:, 0:64], pattern=[[0, 64]], compare_op=ALU.is_ge,
                                             fill=0.0, base=63, channel_multiplier=-1), reads=[B], writes=[B])
    P.op('gpsimd', lambda e: e.affine_select(out=B[:, 64:128], in_=B[:, 64:128], pattern=[[0, 64]], compare_op=ALU.is_ge,
                                             fill=0.0, base=-64, channel_multiplier=1), reads=[B], writes=[B])
    res = {'B': B}
    specs = {'S0': (1, -1, ALU.is_gt), 'I0': (1, -1, ALU.is_ge), 'S1': (-1, 1, ALU.is_gt), 'I1': (-1, 1, ALU.is_ge)}
    for nm, (st, cm, cmp) in specs.items():
        Mt = mk("m_" + nm)
        P.op('gpsimd', lambda e, Mt=Mt, st=st, cm=cm, cmp=cmp: e.affine_select(
            out=Mt[:, :], in_=B[:, :], pattern=[[st, 128]], compare_op=cmp, fill=0.0, base=0, channel_multiplier=cm),
            reads=[B], writes=[Mt])
        res[nm] = Mt
    for d in range(2):
        M2 = mk("m_M2%d" % d, 256)
        P.copy('gpsimd', M2[:, 0:128], res['S%d' % d][:, :], reads=[res['S%d' % d]], writes=[M2])
        P.copy('gpsimd', M2[:, 128:256], res['I%d' % d][:, :], reads=[res['I%d' % d]], writes=[M2])
        res['M2%d' % d] = M2
    return res


def _interleave(*gens):
    gens = [g for g in gens if g is not None]
    while gens:
        for g in list(gens):
            try:
                next(g)
            except StopIteration:
                gens.remove(g)


def _interleave_w(*pairs):
    st = [[g, float(n), 0] for g, n in pairs if g is not None]
    while st:
        st.sort(key=lambda t: t[2] / t[1])
        t = st[0]
        try:
            next(t[0])
            t[2] += 1
        except StopIteration:
            st.remove(t)


def phase2(P, io, ps, ident, only=None):
    Pj, SC, GB = io['Pj'], io['SC'], io['GB']
    ring = PsRing(ps)
    with ExitStack() as les:
        def tl(name, shape=(128, 512)):
            return P.tile("p2_" + name, list(shape), es=les)
        MK = build_masks(P, les)
        mup, mun = tl("mup", (128, RC)), tl("mun", (128, RC))
        P.dma('sync', [(mup.ap(), bc(io['mu_prev']))], writes=[mup])
        P.dma('sync', [(mun.ap(), bc(io['mu_next']))], writes=[mun])
        w0b, a0b, wup, aup = [], [], [], []
        for d in range(2):
            t_ = tl("w0b%d" % d); P.dma('sync', [(t_.ap(), bc(io['w_decay0'][d:d + 1, :]))], writes=[t_]); w0b.append(t_)
            t_ = tl("a0b%d" % d); P.dma('sync', [(t_.ap(), bc(io['a_gate0'][d:d + 1, :]))], writes=[t_]); a0b.append(t_)
            t_ = tl("wup%d" % d, (64, 512)); P.dma('sync', [(t_.ap(), io['w_decay_up'][d])], writes=[t_]); wup.append(t_)
            t_ = tl("aup%d" % d, (64, 512)); P.dma('sync', [(t_.ap(), io['a_gate_up'][d])], writes=[t_]); aup.append(t_)
        gup = tl("gup"); P.dma('sync', [(gup.ap(), io['g_up'])], writes=[gup])
        kab, omka, kkb, rkb = tl("kab"), tl("omka"), tl("kkb"), tl("rkb")
        P.dma('sync', [(kab.ap(), bc(io['k_a']))], writes=[kab])
        P.dma('sync', [(kkb.ap(), bc(io['k_k']))], writes=[kkb])
        P.dma('sync', [(rkb.ap(), bc(io['r_k']))], writes=[rkb])
        P.ts('vector', omka[:, :], kab[:, :], -1.0, 1.0, ALU.mult, ALU.add, reads=[kab], writes=[omka])

        HC = RC // 2
        uh = [[tl("%s%d" % (n, k), (128, HC)) for k in range(2)] for n in ("u", "up", "un", "t1")]
        us = tl("us", (128, RC))
        xl = tl("xl", (128, 256))
        lT = tl("lT", (128, 384))
        lw = [tl("lw%d" % d) for d in range(2)]
        ad = [tl("ad%d" % d) for d in range(2)]
        kd = [tl("kd%d" % d) for d in range(2)]
        bd = [tl("bd%d" % d) for d in range(2)]
        At, Rt, Bt, Kt, BH, KH = tl("At"), tl("Rt"), tl("Bt"), tl("Kt"), tl("BH"), tl("KH")
        e_incl, e_inv, e_excl, e_rem, etot, elw = tl("e_incl"), tl("e_inv"), tl("e_excl"), tl("e_rem"), tl("etot"), tl("elw")
        tmp = [elw, etot, Kt, BH]
        gt, bon, kkt, kkn = At, Rt, Bt, tl("kkn")
        ssk, inv, bs0, bs1 = tl("ssk", (128, 8)), tl("inv", (128, 8)), tl("bs0", (128, 8)), tl("bs1", (128, 8))
        FAR = tl("FAR", (64, 8, 256))
        FBK = tl("FBK", (64, 8, 256))
        MKt = tl("MKt", (128, 8, 256))
        gam = tl("gam", (64, 16))
        MBt = [tl("MBt%d" % d, (128, 8, 256)) for d in range(2)]
        XTa = [tl("XTa%d" % d, (128, 8, 128)) for d in range(2)]
        Xa = [tl("Xa%d" % d, (128, 8, 128)) for d in range(2)]
        Xb = [tl("Xb%d" % d, (128, 8, 128)) for d in range(2)]
        XTb = [tl("XTb%d" % d, (128, 8, 128)) for d in range(2)]
        Nacc = [tl("Nacc%d" % d, (128, 8, 128)) for d in range(2)]

        def v3(ap, a):
            return ap.rearrange("p (a b) -> p a b", a=a)

        def g_prep(b, i):
            n0 = b * T + i * 128
            for k in range(2):
                cs = slice(k * HC, (k + 1) * HC)
                u, up, un, t1 = uh[0][k], uh[1][k], uh[2][k], uh[3][k]
                P.dma('sync', [(u.ap(), Pj[n0:n0 + 128, cs])], writes=[u])
                if i == 0:
                    P.memset('gpsimd', up, up[:, :], 0.0)
                    P.dma('sync', [(up[1:128, :], Pj[n0:n0 + 127, cs])], writes=[up])
                else:
                    P.dma('sync', [(up.ap(), Pj[n0 - 1:n0 + 127, cs])], writes=[up])
                if i == 15:
                    P.memset('gpsimd', un, un[:, :], 0.0)
                    P.dma('sync', [(un[0:127, :], Pj[n0 + 1:n0 + 128, cs])], writes=[un])
                else:
                    P.dma('sync', [(un.ap(), Pj[n0 + 1:n0 + 129, cs])], writes=[un])
            yield
            for k in range(2):
                cs = slice(k * HC, (k + 1) * HC)
                u, up, un, t1 = uh[0][k], uh[1][k], uh[2][k], uh[3][k]
                P.tt('gpsimd', t1[:, :], up[:, :], u[:, :], ALU.subtract, reads=[up, u], writes=[t1])
                P.tt('gpsimd', t1[:, :], t1[:, :], mup[:, cs], ALU.mult, reads=[t1, mup], writes=[t1])
                P.tt('gpsimd', up[:, :], un[:, :], u[:, :], ALU.subtract, reads=[un, u], writes=[up])
                P.tt('gpsimd', up[:, :], up[:, :], mun[:, cs], ALU.mult, reads=[up, mun], writes=[up])
                P.tt('vector', us[:, cs], u[:, :], t1[:, :], ALU.add, reads=[u, t1], writes=[us])
                P.tt('vector', us[:, cs], us[:, cs], up[:, :], ALU.add, reads=[us, up], writes=[us])
                yield
            r_, k_, v_ = us[:, 0:512], us[:, 512:1024], us[:, 1024:1536]
            P.act(xl[:, 0:64], us[:, 1536:1600], AF.Tanh, reads=[us], writes=[xl])
            P.act(xl[:, 128:256], us[:, 1664:1792], AF.Sigmoid, reads=[us], writes=[xl])
            pA = ring.next()
            P.transpose(pA, pA[0:64, 0:128], xl[:, 0:64], ident, reads=[xl])
            P.transpose(pA, pA[0:64, 128:256], us[:, 1600:1664], ident, reads=[us])
            P.transpose(pA, pA[:, 256:384], xl[:, 128:256], ident, reads=[xl])
            P.copy('vector', lT[0:64, 0:256], pA[0:64, 0:256], reads=[pA], writes=[lT])
            P.copy('vector', lT[:, 256:384], pA[:, 256:384], reads=[pA], writes=[lT])
            yield
            for d in range(2):
                pz = ring.next()
                P.mm(pz, pz[:, :], lT[0:64, 0:128], wup[d][:, :], reads=[lT, wup[d]])
                P.tt('vector', tmp[0][:, :], pz[:, :], w0b[d][:, :], ALU.add, reads=[pz, w0b[d]], writes=[tmp[0]])
                P.act(tmp[0][:, :], tmp[0][:, :], AF.Sigmoid, reads=[tmp[0]], writes=[tmp[0]])
                P.ts('vector', lw[d][:, :], tmp[0][:, :], -0.6065306597126334, None, ALU.mult, reads=[tmp[0]], writes=[lw[d]])
                pz = ring.next()
                P.mm(pz, pz[:, :], lT[0:64, 128:256], aup[d][:, :], reads=[lT, aup[d]])
                P.tt('vector', tmp[1][:, :], pz[:, :], a0b[d][:, :], ALU.add, reads=[pz, a0b[d]], writes=[tmp[1]])
                P.act(ad[d][:, :], tmp[1][:, :], AF.Sigmoid, reads=[tmp[1]], writes=[ad[d]])
                yield
            pz = ring.next()
            P.mm(pz, pz[:, :], lT[:, 256:384], gup[:, :], reads=[lT, gup])
            P.copy('scalar', gt[:, :], pz[:, :], reads=[pz], writes=[gt])
            P.dma('sync', [(GB[n0:n0 + 128, 0, :], gt.ap())], reads=[gt])
            P.tt('vector', kkt[:, :], k_, kkb[:, :], ALU.mult, reads=[us, kkb], writes=[kkt])
            P.tt('gpsimd', tmp[2][:, :], kkt[:, :], kkt[:, :], ALU.mult, reads=[kkt], writes=[tmp[2]])
            P.op('vector', lambda e: e.tensor_reduce(out=ssk[:, :], in_=v3(tmp[2][:, :], 8), axis=AX.X, op=ALU.add),
                 reads=[tmp[2]], writes=[ssk])
            mh = CONST['mhalf']
            P.tt('gpsimd', inv[:, :], ssk[:, :], mh[:, 0:8], ALU.pow, reads=[ssk, mh], writes=[inv])
            P.ts('vector', inv[:, :], inv[:, :], 1e12, None, ALU.min, reads=[inv], writes=[inv])
            P.tt('vector', v3(kkn[:, :], 8), v3(kkt[:, :], 8), inv[:, :].unsqueeze(2).to_broadcast([128, 8, 64]), ALU.mult,
                 reads=[kkt, inv], writes=[kkn])
            yield
            bs = [bs0, bs1]
            for d in range(2):
                P.tt('gpsimd', tmp[3][:, :], ad[d][:, :], kab[:, :], ALU.mult, reads=[ad[d], kab], writes=[tmp[3]])
                P.tt('gpsimd', tmp[3][:, :], tmp[3][:, :], omka[:, :], ALU.add, reads=[tmp[3], omka], writes=[tmp[3]])
                P.tt('vector', kd[d][:, :], k_, tmp[3][:, :], ALU.mult, reads=[us, tmp[3]], writes=[kd[d]])
                P.tt('gpsimd', bd[d][:, :], kkn[:, :], ad[d][:, :], ALU.mult, reads=[kkn, ad[d]], writes=[bd[d]])
                P.tt('vector', tmp[2][:, :], r_, kd[d][:, :], ALU.mult, reads=[us, kd[d]], writes=[tmp[2]])
                P.tt('vector', tmp[2][:, :], tmp[2][:, :], rkb[:, :], ALU.mult, reads=[tmp[2], rkb], writes=[tmp[2]])
                P.op('vector', lambda e, d=d: e.tensor_reduce(out=bs[d][:, :], in_=v3(tmp[2][:, :], 8), axis=AX.X, op=ALU.add),
                     reads=[tmp[2]], writes=[bs[d]])
                yield
            P.tt('vector', bs0[:, :], bs0[:, :], bs1[:, :], ALU.add, reads=[bs0, bs1], writes=[bs0])
            P.tt('vector', v3(bon[:, :], 8), v3(v_, 8), bs0[:, :].unsqueeze(2).to_broadcast([128, 8, 64]), ALU.mult,
                 reads=[us, bs0], writes=[bon])
            P.dma('sync', [(GB[n0:n0 + 128, 1, :], bon.ap())], reads=[bon])
            yield

        def g_early(b, i, d):
            r0 = i * 128
            SCd = SC[b, d]
            r_, v_ = us[:, 0:512], us[:, 1024:1536]
            MB = MBt[d]
            P.dma('sync', [(SCd[r0:r0 + 128, C_V:C_V + 512], v_)], reads=[us], tile=us)
            pc, pt = ring.next(), ring.next()
            P.mm(pc, pc[:, :], MK['I%d' % d][:, :], lw[d][:, :], reads=[MK['I%d' % d], lw[d]])
            P.mm(pt, pt[:, :], MK['B'][:, :], lw[d][:, :], reads=[MK['B'], lw[d]])
            P.act(e_incl[:, :], pc[:, :], AF.Exp, reads=[pc], writes=[e_incl])
            P.act(e_inv[:, :], pc[:, :], AF.Exp, reads=[pc], writes=[e_inv], scale=-1.0)
            P.act(elw[:, :], lw[d][:, :], AF.Exp, reads=[lw[d]], writes=[elw], scale=-1.0)
            P.act(etot[:, :], pt[:, :], AF.Exp, reads=[pt], writes=[etot])
            yield
            P.tt('gpsimd', e_excl[:, :], e_incl[:, :], elw[:, :], ALU.mult, reads=[e_incl, elw], writes=[e_excl])
            P.tt('gpsimd', e_rem[:, :], etot[:, :], e_inv[:, :], ALU.mult, reads=[etot, e_inv], writes=[e_rem])
            P.tt('vector', Rt[:, :], r_, e_incl[:, :], ALU.mult, reads=[us, e_incl], writes=[Rt])
            P.tt('vector', Kt[:, :], kd[d][:, :], e_inv[:, :], ALU.mult, reads=[kd[d], e_inv], writes=[Kt])
            P.tt('gpsimd', Bt[:, :], bd[d][:, :], e_inv[:, :], ALU.mult, reads=[bd[d], e_inv], writes=[Bt])
            P.op('vector', lambda e: e.scalar_tensor_tensor(out=At[:, :], in0=kkn[:, :], scalar=-1.0, in1=e_excl[:, :],
                                                            op0=ALU.mult, op1=ALU.mult),
                 reads=[kkn, e_excl], writes=[At])
            yield
            P.tt('gpsimd', BH[:, :], bd[d][:, :], e_rem[:, :], ALU.mult, reads=[bd[d], e_rem], writes=[BH])
            P.tt('gpsimd', KH[:, :], kd[d][:, :], e_rem[:, :], ALU.mult, reads=[kd[d], e_rem], writes=[KH])
            P.dma('sync', [(SCd[r0:r0 + 128, C_BH:C_BH + 512], BH.ap())], reads=[BH])
            P.dma('sync', [(SCd[r0:r0 + 128, C_KH:C_KH + 512], KH.ap())], reads=[KH])
            pg = ring.next()
            for hh in range(8):
                P.mm(pg, pg[0:64, hh * 2:hh * 2 + 2], lw[d][:, hh * 64:(hh + 1) * 64], MK['B'][:, 0:128:64],
                     reads=[lw[d], MK['B']])
            P.act(gam[:, :].rearrange("p (c h) -> p h c", c=2), pg[0:64, 0:16].rearrange("p (h c) -> p h c", c=2), AF.Exp,
                  reads=[pg], writes=[gam])
            for half in range(2):
                rr = r0 + half * 64
                P.dma('sync', [(SCd[rr:rr + 64, C_GAM:C_GAM + 8], gam[:, half * 8:half * 8 + 8])], reads=[gam])
            yield
            for (src, dst, off) in ((At, FAR, 0), (Rt, FAR, 128), (Bt, FBK, 0), (Kt, FBK, 128)):
                for hg in range(2):
                    pq = ring.next()
                    for q in range(4):
                        hh = hg * 4 + q
                        P.transpose(pq, pq[0:64, q * 128:(q + 1) * 128], src[:, hh * 64:(hh + 1) * 64], ident, reads=[src])
                    P.copy('scalar' if hg else 'vector', dst[:, hg * 4:hg * 4 + 4, off:off + 128],
                           v3(pq[0:64, :], 4), reads=[pq], writes=[dst])
                yield
            for half in range(2):
                rr = r0 + half * 64
                P.dma('sync', [(v3(SCd[rr:rr + 64, C_FA:C_FA + 512], 8), FAR[:, :, half * 64:half * 64 + 64]),
                               (v3(SCd[rr:rr + 64, C_FR:C_FR + 512], 8), FAR[:, :, 128 + half * 64:192 + half * 64])],
                      reads=[FAR])
            for (off, Mt_) in ((128, MKt), (0, MB)):
                for hp in range(4):
                    pq = ring.next()
                    for q in range(2):
                        hh = hp * 2 + q
                        P.mm(pq, pq[:, q * 256:(q + 1) * 256], FBK[:, hh, off:off + 128], FAR[:, hh, :], reads=[FBK, FAR])
                    P.tt('vector', Mt_[:, hp * 2:hp * 2 + 2, :], v3(pq[:, :], 2),
                         MK['M2%d' % d][:, :].unsqueeze(1).to_broadcast([128, 2, 256]), ALU.mult,
                         reads=[pq, MK['M2%d' % d]], writes=[Mt_])
                    if hp % 2:
                        yield
            for hg in range(2):
                pq = ring.next()
                for q in range(4):
                    hh = hg * 4 + q
                    P.mm(pq, pq[:, q * 128:(q + 1) * 128], FAR[:, hh, 0:128], FBK[:, hh, 0:128], reads=[FBK, FAR])
                P.tt('vector', XTa[d][:, hg * 4:hg * 4 + 4, :], v3(pq[:, :], 4),
                     MK['S%d' % (1 - d)][:, :].unsqueeze(1).to_broadcast([128, 4, 128]), ALU.mult,
                     reads=[pq, MK['S%d' % (1 - d)]], writes=[XTa[d]])
            yield
            for half in range(2):
                rr = r0 + half * 64
                hs = slice(half * 64, half * 64 + 64)
                P.dma('sync', [(v3(SCd[rr:rr + 64, C_MAK:C_MAK + 512], 8), MKt[hs, :, half * 64:half * 64 + 64]),
                               (v3(SCd[rr:rr + 64, C_MRK:C_MRK + 512], 8), MKt[hs, :, 128 + half * 64:192 + half * 64])],
                      reads=[MKt])
                P.dma('sync', [(v3(SCd[rr:rr + 64, C_MRB:C_MRB + 512], 8), MB[hs, :, 128 + half * 64:192 + half * 64])],
                      reads=[MB])
            yield

        def g_inv(b, i, d):
            r0 = i * 128
            SCd = SC[b, d]
            MB, Na = MBt[d], Nacc[d]
            P.tt('vector', Na[:, :, :], MB[:, :, 0:128], ident[:, :].unsqueeze(1).to_broadcast([128, 8, 128]), ALU.add,
                 reads=[MB, ident], writes=[Na])
            X_ap, X_t = (lambda hh: MB[:, hh, 0:128]), MB
            XT_t = XTa[d]
            outs = [(Xa[d], XTb[d]), (Xb[d], XTa[d]), (Xa[d], XTb[d]), (Xb[d], XTa[d]), (Xa[d], XTb[d])]
            for kq in range(5):
                Xn_t, XTn_t = outs[kq]
                for hg in range(2):
                    if kq < 4:
                        pq = ring.next()
                        for q in range(4):
                            hh = hg * 4 + q
                            P.mm(pq, pq[:, q * 128:(q + 1) * 128], XT_t[:, hh, :], X_ap(hh), reads=[XT_t, X_t])
                        P.copy('scalar', Xn_t[:, hg * 4:hg * 4 + 4, :], v3(pq[:, :], 4), reads=[pq], writes=[Xn_t])
                    pq = ring.next()
                    for q in range(4):
                        hh = hg * 4 + q
                        P.mm(pq, pq[:, q * 128:(q + 1) * 128], X_ap(hh), XT_t[:, hh, :], reads=[XT_t, X_t])
                    P.copy('scalar', XTn_t[:, hg * 4:hg * 4 + 4, :], v3(pq[:, :], 4), reads=[pq], writes=[XTn_t])
                    yield
                for hg in range(2):
                    pq = ring.next()
                    for q in range(4):
                        hh = hg * 4 + q
                        P.mm(pq, pq[:, q * 128:(q + 1) * 128], XTn_t[:, hh, :], Na[:, hh, :], reads=[XTn_t, Na])
                    P.tt('vector', Na[:, hg * 4:hg * 4 + 4, :], v3(pq[:, :], 4), Na[:, hg * 4:hg * 4 + 4, :], ALU.add,
                         reads=[pq, Na], writes=[Na])
                    yield
                X_t = Xn_t
                X_ap = (lambda hh, X_t=X_t: X_t[:, hh, :])
                XT_t = XTn_t
            for half in range(2):
                rr = r0 + half * 64
                hs = slice(half * 64, half * 64 + 64)
                P.dma('sync', [(v3(SCd[rr:rr + 64, C_N:C_N + 512], 8), Na[hs, :, half * 64:half * 64 + 64])], reads=[Na])
            yield

        def chain(*gs):
            for g in gs:
                yield from g
        tiles = [(b, i) for b in range(NB) for i in range(16) if only is None or (b, i) in only]
        if tiles:
            _interleave(chain(g_prep(*tiles[0]), g_early(*tiles[0], 0)))
        for ti, (b, i) in enumerate(tiles):
            _interleave(g_early(b, i, 1), g_inv(b, i, 0))
            nxt = tiles[ti + 1] if ti + 1 < len(tiles) else None
            _interleave(chain(g_prep(*nxt), g_early(*nxt, 0)) if nxt else None, g_inv(b, i, 1))


def phase3_gen(P, io, ps, les, nsteps=32):
    SC, YD = io['SC'], io['YD']

    def tl(name, shape=(128, 512)):
        return P.tile("p3_" + name, list(shape), es=les)
    G = {}
    for b in range(NB):
        G[b] = dict(rec=[tl("rec%d_%d" % (b, k), (128, NCOL)) for k in range(2)], S=tl("S%d" % b),
                    XT=tl("XT%d" % b), UT=tl("UT%d" % b), Y=[tl("Y%d_%d" % (b, k)) for k in range(2)],
                    pA=[ps[4 * b], ps[4 * b + 1]], pB=[ps[4 * b + 2], ps[4 * b + 3]])
        P.memset('vector', G[b]['S'], G[b]['S'][:, :], 0.0)

    def load(b, s):
        R = G[b]['rec'][s % 2]
        pairs = []
        for d in range(2):
            c = s if d == 0 else 31 - s
            pairs.append((R[d * 64:(d + 1) * 64, :], SC[b, d, c * 64:(c + 1) * 64, :]))
        P.dma('sync', pairs, writes=[R])
    for b in range(NB):
        load(b, 0)
    for s in range(nsteps):
        for b in range(NB):
            g = G[b]
            if s + 1 < nsteps:
                load(b, s + 1)
            R, S, XT, UT = g['rec'][s % 2], g['S'], g['XT'], g['UT']
            Y = g['Y'][s % 2]
            for d in range(2):
                p0 = d * 64
                pr = slice(p0, p0 + 64)
                pA, pB = g['pA'][d], g['pB'][d]

                def hs(base, h):
                    return R[pr, base + h * 64:base + (h + 1) * 64]

                def sl(t_, h):
                    return t_[pr, h * 64:(h + 1) * 64]
                for h in range(8):
                    P.mm(pA, sl(pA, h), hs(C_FA, h), sl(S, h), start=True, stop=False, reads=[R, S])
                    P.mm(pA, sl(pA, h), hs(C_MAK, h), hs(C_V, h), start=False, stop=True, reads=[R])
                P.copy('scalar', XT[pr, :], pA[pr, :], reads=[pA], writes=[XT])
            for d in range(2):
                p0 = d * 64
                pr = slice(p0, p0 + 64)
                pA, pB = g['pA'][d], g['pB'][d]

                def hs(base, h):
                    return R[pr, base + h * 64:base + (h + 1) * 64]

                def sl(t_, h):
                    return t_[pr, h * 64:(h + 1) * 64]
                for h in range(8):
                    P.mm(pB, sl(pB, h), hs(C_N, h), sl(XT, h), reads=[R, XT])
                P.copy('vector', UT[pr, :], pB[pr, :], reads=[pB], writes=[UT])
            for d in range(2):
                p0 = d * 64
                pr = slice(p0, p0 + 64)
                pA, pB = g['pA'][d], g['pB'][d]
                c = s if d == 0 else 31 - s

                def hs(base, h):
                    return R[pr, base + h * 64:base + (h + 1) * 64]

                def sl(t_, h):
                    return t_[pr, h * 64:(h + 1) * 64]
                for h in range(8):
                    P.mm(pA, sl(pA, h), hs(C_FR, h), sl(S, h), start=True, stop=False, reads=[R, S])
                    P.mm(pA, sl(pA, h), hs(C_MRB, h), sl(UT, h), start=False, stop=False, reads=[R, UT])
                    P.mm(pA, sl(pA, h), hs(C_MRK, h), hs(C_V, h), start=False, stop=True, reads=[R])
                for h in range(8):
                    P.mm(pB, sl(pB, h), hs(C_BH, h), sl(UT, h), start=True, stop=False, reads=[R, UT])
                    P.mm(pB, sl(pB, h), hs(C_KH, h), hs(C_V, h), start=False, stop=True, reads=[R])
                P.copy('scalar', Y[pr, :], pA[pr, :], reads=[pA], writes=[Y])
                n0 = b * T + c * 64
                P.dma('sync', [(YD[d, n0:n0 + 64, :], Y[pr, :])], reads=[Y])
                P.tt('vector', S[pr, :].rearrange("p (h i) -> p h i", h=8), S[pr, :].rearrange("p (h i) -> p h i", h=8),
                     R[pr, C_GAM:C_GAM + 8].unsqueeze(2).to_broadcast([64, 8, 64]), ALU.mult, reads=[S, R], writes=[S])
                P.tt('vector', S[pr, :], pB[pr, :], S[pr, :], ALU.add, reads=[pB, S], writes=[S])
            yield


def phase3(P, io, ps, nsteps=32):
    with ExitStack() as les:
        _interleave(phase3_gen(P, io, ps, les, nsteps))


def phase4(P, io, ps, ident, only=None):
    with ExitStack() as les:
        _interleave(phase4_gen(P, io, ps, ident, les, only))


def phase4_gen(P, io, ps, ident, les, only=None):
    YD, GB, Pj, MR = io['YD'], io['GB'], io['Pj'], io['MR']
    ring = PsRing(ps)
    if True:
        def tl(name, shape=(128, 512)):
            return P.tile("p4_" + name, list(shape), es=les)
        gnw, gnb = tl("gnw"), tl("gnb")
        P.dma('sync', [(gnw.ap(), bc(io['gn_w']))], writes=[gnw])
        P.dma('sync', [(gnb.ap(), bc(io['gn_b']))], writes=[gnb])
        wro = tl("wro", (128, 4, D))
        P.dma('sync', [(wro.ap(), io['w_rwkv_out'].rearrange("(c p) n -> p c n", p=128))], writes=[wro])
        y0, y1, gb2 = tl("y0"), tl("y1"), tl("gb2", (128, 2, 512))
        xc, sq, yr = tl("xc"), tl("sq"), tl("yr")
        s1, s2 = tl("s1", (128, 8)), tl("s2", (128, 8))
        yrT = tl("yrT", (128, 4, 128))
        mr = tl("mr", (128, D))

        def v3(ap, a=8):
            return ap.rearrange("p (a b) -> p a b", a=a)

        def bc8(t_):
            return t_[:, :].unsqueeze(2).to_broadcast([128, 8, 64])
        for i in range(NTILE):
            if only is not None and i not in only:
                continue
            n0 = i * 128
            P.dma('sync', [(y0.ap(), YD[0, n0:n0 + 128, :])], writes=[y0])
            P.dma('sync', [(y1.ap(), YD[1, n0:n0 + 128, :])], writes=[y1])
            P.dma('sync', [(gb2.ap(), GB[n0:n0 + 128, :, :])], writes=[gb2])
            P.tt('vector', y0[:, :], y0[:, :], y1[:, :], ALU.add, reads=[y0, y1], writes=[y0])
            P.op('vector', lambda e: e.tensor_reduce(out=s1[:, :], in_=v3(y0[:, :]), axis=AX.X, op=ALU.add), reads=[y0], writes=[s1])
            P.ts('vector', s1[:, :], s1[:, :], 1.0 / 64, None, ALU.mult, reads=[s1], writes=[s1])
            P.tt('vector', v3(xc[:, :]), v3(y0[:, :]), bc8(s1), ALU.subtract, reads=[y0, s1], writes=[xc])
            yield
            P.tt('gpsimd', sq[:, :], xc[:, :], xc[:, :], ALU.mult, reads=[xc], writes=[sq])
            P.op('vector', lambda e: e.tensor_reduce(out=s2[:, :], in_=v3(sq[:, :]), axis=AX.X, op=ALU.add), reads=[sq], writes=[s2])
            P.ts('vector', s2[:, :], s2[:, :], 1.0 / 64, 64e-5, ALU.mult, ALU.add, reads=[s2], writes=[s2])
            mh = CONST['mhalf']
            P.tt('gpsimd', s2[:, :], s2[:, :], mh[:, 0:8], ALU.pow, reads=[s2, mh], writes=[s2])
            yield
            P.tt('vector', v3(xc[:, :]), v3(xc[:, :]), bc8(s2), ALU.mult, reads=[xc, s2], writes=[xc])
            P.tt('gpsimd', xc[:, :], xc[:, :], gnw[:, :], ALU.mult, reads=[xc, gnw], writes=[xc])
            P.tt('gpsimd', xc[:, :], xc[:, :], gnb[:, :], ALU.add, reads=[xc, gnb], writes=[xc])
            yield
            P.tt('vector', xc[:, :], xc[:, :], gb2[:, 1, :], ALU.add, reads=[xc, gb2], writes=[xc])
            P.tt('vector', yr[:, :], xc[:, :], gb2[:, 0, :], ALU.mult, reads=[xc, gb2], writes=[yr])
            yield
            if 'YR' in io:
                P.dma('sync', [(io['YR'][n0:n0 + 128, :], yr.ap())], reads=[yr])
            pq = ring.next()
            for c in range(4):
                P.transpose(pq, pq[:, c * 128:(c + 1) * 128], yr[:, c * 128:(c + 1) * 128], ident, reads=[yr])
            P.copy('scalar', yrT[:, :, :], pq[:, :].rearrange("p (c t) -> p c t", c=4), reads=[pq], writes=[yrT])
            yield
            for hf in range(2):
                pq = ring.next()
                for c in range(4):
                    P.mm(pq, pq[:, :], yrT[:, c, :], wro[:, c, hf * 512:(hf + 1) * 512], start=(c == 0), stop=(c == 3), reads=[yrT, wro])
                P.copy('vector' if hf else 'scalar', mr[:, hf * 512:(hf + 1) * 512], pq[:, :], reads=[pq], writes=[mr])
                yield
            P.dma('sync', [(MR[n0:n0 + 128, :], mr.ap())], reads=[mr])
            yield


IN_SPECS = [
    ('x', [NT, D], F32), ('pos', [NT, 1], I32), ('norm1_g', [1, D], F32), ('w_in', [D, INC], F32),
    ('mu_prev', [1, RC], F32), ('mu_next', [1, RC], F32), ('w_decay0', [2, 512], F32),
    ('w_decay_up', [2, 64, 512], F32), ('a_gate0', [2, 512], F32), ('a_gate_up', [2, 64, 512], F32),
    ('g_up', [128, 512], F32), ('k_k', [1, 512], F32), ('k_a', [1, 512], F32), ('r_k', [1, 512], F32),
    ('gn_w', [1, 512], F32), ('gn_b', [1, 512], F32), ('w_rwkv_out', [512, D], F32),
    ('q_norm_g', [1, 64], F32), ('k_norm_g', [1, 64], F32), ('w_attn_out', [256, D], F32),
    ('b_merge', [2, D], F32), ('w_out', [D, D], F32), ('norm2_g', [1, D], F32),
    ('w_router', [D, 36], F32), ('b_router', [1, 36], F32),
    ('w_gate', [32 * D, 512], F32), ('w_up', [32 * D, 512], F32), ('w_down', [32 * 512, D], F32),
]
SCRATCH = [
    ('Pj', [NT, INC]), ('SC', [NB, 2, T, NCOL]), ('GB', [NT, 2, 512]), ('YD', [2, NT, 512]), ('MR', [NT, D]),
    ('QKV', [NT, AC]), ('OA', [3, NT, 260]), ('X1', [NT, D]), ('XE', [NROWS, D]), ('YE', [NROWS, D]),
]


def build_program(phases, dbg_out=(), dbg_in=(), opts=None):
    opts = opts or {}
    nc = bass.Bass("TRN2", target_bir_lowering=False)
    io = {}
    for name, shape, dt in IN_SPECS:
        if opts.get('inputs') is not None and name not in opts['inputs']:
            continue
        io[name] = nc.dram_tensor(name, list(shape), dt, kind="ExternalInput").ap()
    for name, shape in SCRATCH + list(opts.get('extra_scratch', [])):
        kind = "ExternalOutput" if name in dbg_out else ("ExternalInput" if name in dbg_in else "Internal")
        io[name] = nc.dram_tensor(name, list(shape), F32, kind=kind).ap()
        io['d_' + name] = Dep()
    io['y'] = nc.dram_tensor("y", [NT, D], F32, kind="ExternalOutput").ap()
    with ExitStack() as es:
        P = Prog(nc, es)
        ident = P.tile("ident", [128, 128])
        P.make_identity(ident)
        ps = [P.psum("ps%d" % i) for i in range(8)]
        RT = P.tile("RT", [128, NTILE, 2])
        CONST['mhalf'] = P.tile("c_mhalf", [128, 24])
        P.memset('gpsimd', CONST['mhalf'], CONST['mhalf'][:, :], -0.5)
        CONST['e'] = P.tile("c_e", [128, 4])
        P.memset('gpsimd', CONST['e'], CONST['e'][:, :], float(np.e))
        RTi = P.tile("RTi", [128, NTILE, 2], I32)
        for ph in phases:
            if ph == 1:
                phase1(P, io, ps, ident)
            elif ph == 2:
                phase2(P, io, ps, ident, only=opts.get('only2'))
            elif ph == 3:
                phase3(P, io, ps, nsteps=opts.get('nsteps', 32))
            elif ph == 35:
                with ExitStack() as les:
                    _interleave_w((phase3_gen(P, io, ps, les, opts.get('nsteps', 32)), 64), (phase5a_gen(P, io, les, opts.get('only5a')), 128))
            elif ph == 50:
                phase5b(P, io, ps, ident, only=opts.get('only5b'))
            elif ph == 45:
                with ExitStack() as les:
                    if 'XE' in io:
                        XE, YE = io['XE'], io['YE']
                        zt = P.tile("p45_zt", [128, 2, D], es=les)
                        P.memset('gpsimd', zt, zt[:, :, :], 0.0)
                        xev = XE[0:32 * CAP, :].rearrange("(a p r) n -> a p r n", p=128, r=2)
                        for a in range(32 * CAP // 256):
                            P.dma('gpsimd', [(xev[a], zt.ap())], reads=[zt])
                        P.dma('gpsimd', [(XE[TRASH:TRASH + 1, :], zt[0:1, 0, :]), (YE[TRASH:TRASH + 1, :], zt[0:1, 0, :])], reads=[zt])
                    g5 = phase5b_gen(P, io, ps[2:8], ident, les, opts.get('only5b'))
                    for _ in range(16):
                        next(g5, None)
                    _interleave(g5, phase4_gen(P, io, ps[0:2], ident, les, opts.get('only4')))
            elif ph == 4:
                phase4(P, io, ps, ident, only=opts.get('only4'))
            elif ph == 5:
                phase5a(P, io, ps, only=opts.get('only5a'))
                P.end_phase()
                phase5b(P, io, ps, ident, only=opts.get('only5b'))
            elif ph == 6:
                phase6(P, io, ps, ident, RT, RTi, only=opts.get('only6'))
            elif ph == 7:
                phase7(P, io, ps, ident, experts=opts.get('experts', range(32)))
            elif ph == 8:
                phase8(P, io, RT, RTi, only=opts.get('only6'))
            if ph == 1:
                P.barrier()
            else:
                P.end_phase()
        P.finish()
    return nc


def make_inputs(inputs, core):
    b0 = core * NB
    f = np.ascontiguousarray
    m = {
        'x': f(inputs['x'][b0:b0 + NB].reshape(NT, D)),
        'pos': f(inputs['positions'][b0:b0 + NB].reshape(NT, 1).astype(np.int32)),
        'w_in': f(inputs['w_in'][0]),
        'w_decay0': f(inputs['w_decay0'][0]), 'w_decay_up': f(inputs['w_decay_up'][0]),
        'a_gate0': f(inputs['a_gate0'][0]), 'a_gate_up': f(inputs['a_gate_up'][0]),
        'g_up': f(inputs['g_up'][0]), 'w_rwkv_out': f(inputs['w_rwkv_out'][0]),
        'w_attn_out': f(inputs['w_attn_out'][0]), 'b_merge': f(inputs['b_merge'][0]), 'w_out': f(inputs['w_out'][0]),
        'w_router': f(np.concatenate([inputs['w_router_group'][0], inputs['w_router_expert'][0]], axis=1)),
        'b_router': f(np.concatenate([inputs['b_router_group'][0], inputs['b_router_expert'][0]], axis=0).reshape(1, 36)),
        'w_gate': f(inputs['w_gate'][0].reshape(32 * D, 512)), 'w_up': f(inputs['w_up'][0].reshape(32 * D, 512)),
        'w_down': f(inputs['w_down'][0].reshape(32 * 512, D)),
    }
    for k in ('norm1_g', 'mu_prev', 'mu_next', 'k_k', 'k_a', 'r_k', 'gn_w', 'gn_b', 'q_norm_g', 'k_norm_g', 'norm2_g'):
        m[k] = f(inputs[k].reshape(1, -1))
    return m


ATT_PAT = ((1, 2048), (4, 512), (16, 128))
TWO_PI_HI = 6.28125
TWO_PI_LO = 2.0 * np.pi - 6.28125
MAGIC = 12582912.0


def phase5a(P, io, ps, only=None):
    with ExitStack() as les:
        _interleave(phase5a_gen(P, io, les, only))


def phase5a_gen(P, io, les, only=None):
    Pj, QKV, pos = io['Pj'], io['QKV'], io['pos']
    if True:
        def tl(name, shape, dt=F32):
            return P.tile("p5a_" + name, list(shape), dt, es=les)
        gq, gk = tl("gq", (128, 64)), tl("gk", (128, 64))
        P.dma('sync', [(gq.ap(), bc(io['q_norm_g']))], writes=[gq])
        P.dma('sync', [(gk.ap(), bc(io['k_norm_g']))], writes=[gk])
        invf = tl("invf", (128, 8))
        for i in range(8):
            P.memset('vector', invf, invf[:, i:i + 1], float(np.float32(500000.0) ** np.float32(-(i * 2.0 / 16))))
        qkv = tl("qkv", (128, AC))
        qo = tl("qo", (128, 1536))
        sq = tl("sq", (128, 1536))
        ss = tl("ss", (128, 24))
        posi = tl("posi", (128, 1), I32)
        posf = tl("posf", (128, 1))
        ang, tA, ks_, kc_, ths, thc, sn, cs = [tl(n, (128, 8)) for n in ("ang", "tA", "ks", "kc", "ths", "thc", "sn", "cs")]
        r1, r2, tm = tl("r1", (128, 24, 8)), tl("r2", (128, 24, 8)), tl("tm", (128, 24, 8))

        def red(theta, k_, add_half_pi):
            P.op('vector', lambda e: e.scalar_tensor_tensor(out=theta[:, :], in0=k_[:, :], scalar=-TWO_PI_HI, in1=ang[:, :],
                                                            op0=ALU.mult, op1=ALU.add), reads=[k_, ang], writes=[theta])
            P.op('vector', lambda e: e.scalar_tensor_tensor(out=theta[:, :], in0=k_[:, :], scalar=-TWO_PI_LO, in1=theta[:, :],
                                                            op0=ALU.mult, op1=ALU.add), reads=[k_, theta], writes=[theta])
            if add_half_pi:
                P.ts('vector', theta[:, :], theta[:, :], float(np.pi / 2), None, ALU.add, reads=[theta], writes=[theta])
            P.ts('vector', theta[:, :], theta[:, :], float(np.pi), float(-np.pi), ALU.min, ALU.max, reads=[theta], writes=[theta])
        for i in range(NTILE):
            if only is not None and i not in only:
                continue
            n0 = i * 128
            P.dma('sync', [(qkv.ap(), Pj[n0:n0 + 128, RC:RC + AC])], writes=[qkv])
            P.dma('sync', [(posi.ap(), pos[n0:n0 + 128, :])], writes=[posi])
            P.copy('vector', posf[:, :], posi[:, :], reads=[posi], writes=[posf])
            P.ts('vector', ang[:, :], invf[:, :], posf[:, 0:1], None, ALU.mult, reads=[invf, posf], writes=[ang])
            P.ts('vector', tA[:, :], ang[:, :], float(1.0 / (2.0 * np.pi)), None, ALU.mult, reads=[ang], writes=[tA])
            P.ts('vector', ks_[:, :], tA[:, :], MAGIC, None, ALU.add, reads=[tA], writes=[ks_])
            P.ts('vector', ks_[:, :], ks_[:, :], -MAGIC, None, ALU.add, reads=[ks_], writes=[ks_])
            P.ts('vector', kc_[:, :], tA[:, :], 0.25, None, ALU.add, reads=[tA], writes=[kc_])
            P.ts('vector', kc_[:, :], kc_[:, :], MAGIC, None, ALU.add, reads=[kc_], writes=[kc_])
            P.ts('vector', kc_[:, :], kc_[:, :], -MAGIC, None, ALU.add, reads=[kc_], writes=[kc_])
            red(ths, ks_, False)
            red(thc, kc_, True)
            P.act(sn[:, :], ths[:, :], AF.Sin, reads=[ths], writes=[sn])
            P.act(cs[:, :], thc[:, :], AF.Sin, reads=[thc], writes=[cs])
            yield
            P.tt('gpsimd', sq[:, :], qkv[:, 0:1536], qkv[:, 0:1536], ALU.mult, reads=[qkv], writes=[sq])
            P.op('vector', lambda e: e.tensor_reduce(out=ss[:, :], in_=sq[:, :].rearrange("p (a b) -> p a b", a=24), axis=AX.X, op=ALU.add),
                 reads=[sq], writes=[ss])
            P.ts('vector', ss[:, :], ss[:, :], 1.0 / 64, 1e-6, ALU.mult, ALU.add, reads=[ss], writes=[ss])
            mh = CONST['mhalf']
            P.tt('gpsimd', ss[:, :], ss[:, :], mh[:, 0:24], ALU.pow, reads=[ss, mh], writes=[ss])
            yield
            P.tt('vector', qo[:, :].rearrange("p (a b) -> p a b", a=24), qkv[:, 0:1536].rearrange("p (a b) -> p a b", a=24),
                 ss[:, :].unsqueeze(2).to_broadcast([128, 24, 64]), ALU.mult, reads=[qkv, ss], writes=[qo])
            for (o, g_) in ((0, gq), (768, gk)):
                P.tt('gpsimd', qo[:, o:o + 768].rearrange("p (a b) -> p a b", a=12), qo[:, o:o + 768].rearrange("p (a b) -> p a b", a=12),
                     g_[:, :].unsqueeze(1).to_broadcast([128, 12, 64]), ALU.mult, reads=[qo, g_], writes=[qo])
            qv = qo[:, :].rearrange("p (a b) -> p a b", a=24)
            t1, t2 = qv[:, :, 0:8], qv[:, :, 8:16]
            csb = cs[:, :].unsqueeze(1).to_broadcast([128, 24, 8])
            snb = sn[:, :].unsqueeze(1).to_broadcast([128, 24, 8])
            yield
            P.tt('vector', r1[:, :, :], t1, csb, ALU.mult, reads=[qo, cs], writes=[r1])
            P.tt('vector', tm[:, :, :], t2, snb, ALU.mult, reads=[qo, sn], writes=[tm])
            P.tt('vector', r1[:, :, :], r1[:, :, :], tm[:, :, :], ALU.subtract, reads=[r1, tm], writes=[r1])
            P.tt('vector', r2[:, :, :], t2, csb, ALU.mult, reads=[qo, cs], writes=[r2])
            P.tt('vector', tm[:, :, :], t1, snb, ALU.mult, reads=[qo, sn], writes=[tm])
            P.tt('vector', r2[:, :, :], r2[:, :, :], tm[:, :, :], ALU.add, reads=[r2, tm], writes=[r2])
            P.copy('vector', t1, r1[:, :, :], reads=[r1], writes=[qo])
            P.copy('vector', t2, r2[:, :, :], reads=[r2], writes=[qo])
            P.dma('sync', [(QKV[n0:n0 + 128, 0:1536], qo.ap())], reads=[qo])
            P.dma('sync', [(QKV[n0:n0 + 128, 1536:2304], qkv[:, 1536:2304])], reads=[qkv], tile=qkv)
            yield


def phase5b(P, io, ps, ident, only=None):
    with ExitStack() as les:
        _interleave(phase5b_gen(P, io, ps, ident, les, only))


def phase5b_gen(P, io, ps, ident, les, only=None):
    QKV, OA = io['QKV'], io['OA']
    ring_o, ring_s, ring_l = PsRing(ps[0:2]), PsRing(ps[2:4]), PsRing(ps[4:6])

    def tl(name, shape, dt=F32):
        return P.tile("p5b_" + name, list(shape), dt, es=les)
    M3 = tl("M3", (128, 3, 128))
    P.memset('gpsimd', M3, M3[:, :, :], 1.0)

    def sel(ap, step, cm, base):
        P.op('gpsimd', lambda e: e.affine_select(out=ap, in_=ap, pattern=[[step, 128]], compare_op=ALU.is_ge, fill=0.0,
                                                 base=base, channel_multiplier=cm), reads=[M3], writes=[M3])
    sel(M3[:, 0, :], -1, 1, -64)
    sel(M3[:, 1, :], -1, 1, 64)
    sel(M3[:, 1, :], 1, -1, 64)
    sel(M3[:, 2, :], 1, -1, -64)
    QT = [tl("QT%d" % k, (128, 2, T)) for k in range(2)]
    KT = [tl("KT%d" % k, (128, 2, T)) for k in range(2)]
    V1 = [tl("V1_%d" % k, (128, 16, 4, 65)) for k in range(2)]
    for k in range(2):
        P.memset('vector', V1[k], V1[k][:, :, :, :], 1.0)
    Qs, Ks = [tl("Qs%d" % k, (128, 256)) for k in range(2)], [tl("Ks%d" % k, (128, 256)) for k in range(2)]
    Pt = [tl("Pt%d" % k, (128, 384)) for k in range(3)]
    Ob = [tl("Ob%d" % k, (128, 260)) for k in range(2)]
    units = [(b, g) for b in range(NB) for g in range(3) if only is None or (b, g) in only]
    cnt = [0]

    def rows(b, g, j):
        Dg, L = ATT_PAT[g]
        r, l0 = (j * 128) // L, (j * 128) % L
        st = b * T + l0 * Dg + r
        return slice(st, st + 127 * Dg + 1, Dg) if Dg > 1 else slice(st, st + 128)

    def g_load(ui):
        b, g = units[ui]
        qt_, kt_, v1 = QT[ui % 2], KT[ui % 2], V1[ui % 2]
        for j in range(16):
            q_, k_ = Qs[j % 2], Ks[j % 2]
            rs = rows(b, g, j)
            P.dma('sync', [(q_.ap(), QKV[rs, g * 256:(g + 1) * 256])], writes=[q_])
            P.dma('sync', [(k_.ap(), QKV[rs, 768 + g * 256:768 + (g + 1) * 256])], writes=[k_])
            P.dma('sync', [(v1[:, j, :, 0:64], QKV[rs, 1536 + g * 256:1536 + (g + 1) * 256].rearrange("p (h d) -> p h d", h=4))],
                  writes=[v1])
            for (src, dst) in ((q_, qt_), (k_, kt_)):
                pq = ring_l.next()
                for pr in range(2):
                    P.transpose(pq, pq[:, pr * 128:(pr + 1) * 128], src[:, pr * 128:(pr + 1) * 128], ident, reads=[src])
                P.copy('scalar' if dst is kt_ else 'vector', dst[:, :, j * 128:(j + 1) * 128],
                       pq[:, 0:256].rearrange("p (a t) -> p a t", a=2), reads=[pq], writes=[dst])
            yield

    def g_comp(ui):
        b, g = units[ui]
        Dg, L = ATT_PAT[g]
        nlt = L // 128
        qt_, kt_, v1 = QT[ui % 2], KT[ui % 2], V1[ui % 2]
        for j in range(16):
            r, qt = j // nlt, j % nlt
            kts = [kt for kt in (qt - 1, qt, qt + 1) if 0 <= kt < nlt]
            mlo = kts[0] - (qt - 1)
            nk = len(kts)
            po = ring_o.next()
            for h in range(4):
                pr, hh = h // 2, h % 2
                hp = slice(hh * 64, hh * 64 + 64)
                pS = ring_s.next()
                for ki, kt in enumerate(kts):
                    jk = r * nlt + kt
                    P.mm(pS, pS[:, ki * 128:(ki + 1) * 128], kt_[hp, pr, jk * 128:(jk + 1) * 128], qt_[hp, pr, j * 128:(j + 1) * 128],
                         reads=[kt_, qt_])
                pt_ = Pt[cnt[0] % 3]
                cnt[0] += 1
                P.act(pt_[:, 0:nk * 128], pS[:, 0:nk * 128], AF.Exp, reads=[pS], writes=[pt_], scale=0.125)
                P.tt('vector', pt_[:, 0:nk * 128].rearrange("p (a b) -> p a b", a=nk),
                     pt_[:, 0:nk * 128].rearrange("p (a b) -> p a b", a=nk), M3[:, mlo:mlo + nk, :], ALU.mult,
                     reads=[pt_, M3], writes=[pt_])
                for ki, kt in enumerate(kts):
                    jk = r * nlt + kt
                    P.mm(po, po[:, h * 65:(h + 1) * 65], pt_[:, ki * 128:(ki + 1) * 128], v1[:, jk, h, :],
                         start=(ki == 0), stop=(ki == nk - 1), reads=[pt_, v1])
                yield
            ob = Ob[j % 2]
            P.copy('scalar', ob[:, :], po[:, 0:260], reads=[po], writes=[ob])
            P.dma('sync', [(OA[g, rows(b, g, j), :], ob.ap())], reads=[ob])
    if units:
        for _ in g_load(0):
            yield
    for ui in range(len(units)):
        active = [g_comp(ui)] + ([g_load(ui + 1)] if ui + 1 < len(units) else [])
        while active:
            for gg in list(active):
                try:
                    next(gg)
                    yield
                except StopIteration:
                    active.remove(gg)


def phase6(P, io, ps, ident, RT, RTi, only=None):
    OA, MR, Pj, x, X1, XE, YE = io['OA'], io['MR'], io['Pj'], io['x'], io['X1'], io['XE'], io['YE']
    ring = PsRing(ps)
    with ExitStack() as les:
        def tl(name, shape, dt=F32):
            return P.tile("p6_" + name, list(shape), dt, es=les)
        wao = tl("wao", (128, 2, D))
        P.dma('sync', [(wao.ap(), io['w_attn_out'].rearrange("(c p) n -> p c n", p=128))], writes=[wao])
        wout = tl("wout", (128, 8, D))
        P.dma('sync', [(wout.ap(), io['w_out'].rearrange("(c p) n -> p c n", p=128))], writes=[wout])
        wr = tl("wr", (128, 8, 36))
        P.dma('sync', [(wr.ap(), io['w_router'].rearrange("(c p) n -> p c n", p=128))], writes=[wr])
        bm1, g2b, brb = tl("bm1", (128, D)), tl("g2b", (128, D)), tl("brb", (128, 36))
        bm0 = tl("bm0", (128, D))
        P.dma('sync', [(bm0.ap(), bc(io['b_merge'][0:1, :]))], writes=[bm0])
        P.dma('sync', [(bm1.ap(), bc(io['b_merge'][1:2, :]))], writes=[bm1])
        P.dma('sync', [(g2b.ap(), bc(io['norm2_g']))], writes=[g2b])
        P.dma('sync', [(brb.ap(), bc(io['b_router']))], writes=[brb])
        Us, ones = tl("Us", (128, 128)), tl("ones", (128, 128))
        P.memset('gpsimd', ones, ones[:, :], 1.0)
        P.op('gpsimd', lambda e: e.affine_select(out=Us[:, :], in_=ones[:, :], pattern=[[1, 128]], compare_op=ALU.is_gt, fill=0.0,
                                                 base=0, channel_multiplier=-1), reads=[ones], writes=[Us])
        ebi = tl("ebi", (128, 32), I32)
        P.op('gpsimd', lambda e: e.iota(ebi[:, :], pattern=[[CAP, 32]], base=-TRASH, channel_multiplier=0), reads=[], writes=[ebi])
        ebase = tl("ebase", (128, 32))
        P.copy('vector', ebase[:, :], ebi[:, :], reads=[ebi], writes=[ebase])
        Rrun = tl("Rrun", (128, 32))
        P.memset('vector', Rrun, Rrun[:, :], 0.0)

        junk = tl("junk", (128, D))

        def mkslot(j, par, base=None):
            B = {}
            sfx = "%d_%d" % (j, par)
            if base is None:
                B['oa'] = tl("oa%d" % j, (128, 3, 260))
                for n in ("mr", "gl1", "gl0", "xt", "mm"):
                    B[n] = tl("%s%d" % (n, j), (128, D))
                B['mT'] = tl("mT%d" % j, (128, 8, 128))
                B['ot'], B['oT'] = tl("ot%d" % j, (128, 256)), tl("oT%d" % j, (128, 2, 128))
                B['rden'] = tl("rden%d" % j, (128, 4))
            else:
                for n in ("oa", "mr", "gl1", "gl0", "xt", "mm", "mT", "ot", "oT", "rden"):
                    B[n] = base[n]
            B['junk'] = junk
            for n in ("x1", "h2"):
                B[n] = tl("%s%s" % (n, sfx), (128, D))
            B['h2T'] = tl("h2T" + sfx, (128, 8, 128))
            B['ss'], B['rstd'] = tl("ss" + sfx, (128, 1)), tl("rstd" + sfx, (128, 1))
            B['Lg'] = tl("Lg" + sfx, (128, 36))
            B['sm'] = {n: tl("%s_%s" % (n, sfx), (128, 1)) for n in ("gmax", "gsum", "grp_p", "m1", "m2", "dm", "e21", "den", "g1w", "g2w", "i1f", "i2f", "v1", "v2")}
            B['goh'], B['gex'] = tl("goh" + sfx, (128, 4)), tl("gex" + sfx, (128, 4))
            for n in ("selv", "A1", "A2", "Aa", "rank", "valid", "slot", "tmp32"):
                B[n] = tl("%s%s" % (n, sfx), (128, 32))
            for n in ("sel", "sel2", "oh1", "oh2"):
                B[n] = tl("%s%s" % (n, sfx), (128, 8))
            return B
        slots = {}
        for j in range(2):
            slots[(j, 0)] = mkslot(j, 0)
            slots[(j, 1)] = mkslot(j, 1, base=slots[(j, 0)])

        def v48(t_):
            return t_[:, :].rearrange("p (g e) -> p g e", g=4)

        def tr8(src, dst):
            for hf in range(2):
                pq = ring.next()
                for q in range(4):
                    c = hf * 4 + q
                    P.transpose(pq, pq[:, q * 128:(q + 1) * 128], src[:, c * 128:(c + 1) * 128], ident, reads=[src])
                P.copy('scalar' if hf else 'vector', dst[:, hf * 4:hf * 4 + 4, :], pq[:, :].rearrange("p (c t) -> p c t", c=4),
                       reads=[pq], writes=[dst])

        def st_a(i, B):
            n0 = i * 128
            oa, mr, gl1, gl0, xt, ot, oT, rden = B['oa'], B['mr'], B['gl1'], B['gl0'], B['xt'], B['ot'], B['oT'], B['rden']
            P.dma('sync', [(oa.ap(), OA[:, n0:n0 + 128, :].rearrange("g p c -> p g c"))], writes=[oa])
            P.dma('sync', [(mr.ap(), MR[n0:n0 + 128, :])], writes=[mr])
            P.dma('sync', [(gl1.ap(), Pj[n0:n0 + 128, RC + AC + D:RC + AC + 2 * D])], writes=[gl1])
            P.dma('sync', [(gl0.ap(), Pj[n0:n0 + 128, RC + AC:RC + AC + D])], writes=[gl0])
            P.dma('sync', [(xt.ap(), x[n0:n0 + 128, :])], writes=[xt])
            P.tt('vector', oa[:, 0, :], oa[:, 0, :], oa[:, 1, :], ALU.add, reads=[oa], writes=[oa])
            P.tt('vector', oa[:, 0, :], oa[:, 0, :], oa[:, 2, :], ALU.add, reads=[oa], writes=[oa])
            ov = oa[:, 0, :].rearrange("p (h c) -> p h c", h=4)
            P.op('vector', lambda e: e.reciprocal(rden[:, :].unsqueeze(2), ov[:, :, 64:65]), reads=[oa], writes=[rden])
            P.tt('vector', ot[:, :].rearrange("p (h c) -> p h c", h=4), ov[:, :, 0:64], rden[:, :].unsqueeze(2).to_broadcast([128, 4, 64]),
                 ALU.mult, reads=[oa, rden], writes=[ot])
            if 'OT' in io:
                P.dma('sync', [(io['OT'][n0:n0 + 128, :], ot.ap())], reads=[ot])
            pq = ring.next()
            for c in range(2):
                P.transpose(pq, pq[:, c * 128:(c + 1) * 128], ot[:, c * 128:(c + 1) * 128], ident, reads=[ot])
            P.copy('vector', oT[:, :, :], pq[:, 0:256].rearrange("p (c t) -> p c t", c=2), reads=[pq], writes=[oT])
            P.tt('vector', gl1[:, :], gl1[:, :], bm1[:, :], ALU.add, reads=[gl1, bm1], writes=[gl1])
            P.act(gl1[:, :], gl1[:, :], AF.Sigmoid, reads=[gl1], writes=[gl1])
            P.tt('vector', gl0[:, :], gl0[:, :], bm0[:, :], ALU.add, reads=[gl0, bm0], writes=[gl0])
            P.act(gl0[:, :], gl0[:, :], AF.Sigmoid, reads=[gl0], writes=[gl0])
            P.tt('vector', mr[:, :], mr[:, :], gl0[:, :], ALU.mult, reads=[mr, gl0], writes=[mr])

        def st_b(i, B):
            oT, gl1, mm_, mr, mT = B['oT'], B['gl1'], B['mm'], B['mr'], B['mT']
            for hf in range(2):
                pq = ring.next()
                for c in range(2):
                    P.mm(pq, pq[:, :], oT[:, c, :], wao[:, c, hf * 512:(hf + 1) * 512], start=(c == 0), stop=(c == 1), reads=[oT, wao])
                P.tt('vector', mm_[:, hf * 512:(hf + 1) * 512], pq[:, :], gl1[:, hf * 512:(hf + 1) * 512], ALU.mult,
                     reads=[pq, gl1], writes=[mm_])
            P.tt('vector', mm_[:, :], mm_[:, :], mr[:, :], ALU.add, reads=[mm_, mr], writes=[mm_])
            tr8(mm_, mT)

        def st_c(i, B):
            n0 = i * 128
            mT, xt, x1, h2, junk, ss, rstd, h2T, Lg = B['mT'], B['xt'], B['x1'], B['h2'], B['junk'], B['ss'], B['rstd'], B['h2T'], B['Lg']
            for hf in range(2):
                pq = ring.next()
                for c in range(8):
                    P.mm(pq, pq[:, :], mT[:, c, :], wout[:, c, hf * 512:(hf + 1) * 512], start=(c == 0), stop=(c == 7), reads=[mT, wout])
                P.tt('vector', x1[:, hf * 512:(hf + 1) * 512], pq[:, :], xt[:, hf * 512:(hf + 1) * 512], ALU.add,
                     reads=[pq, xt], writes=[x1])
            P.dma('sync', [(X1[n0:n0 + 128, :], x1.ap())], reads=[x1])
            rmsnorm_tile(P, x1, g2b, h2, junk, ss, rstd, D, 1e-6)
            if 'H2' in io:
                P.dma('sync', [(io['H2'][n0:n0 + 128, :], h2.ap())], reads=[h2])
            tr8(h2, h2T)
            pq = ring.next()
            for c in range(8):
                P.mm(pq, pq[:, 0:36], h2T[:, c, :], wr[:, c, :], start=(c == 0), stop=(c == 7), reads=[h2T, wr])
            P.tt('vector', Lg[:, :], pq[:, 0:36], brb[:, :], ALU.add, reads=[pq, brb], writes=[Lg])
            if 'LG' in io:
                P.dma('sync', [(io['LG'][n0:n0 + 128, :], Lg.ap())], reads=[Lg])

        def st_d(i, B):
            S_, Lg, goh, gex, selv, sel, sel2, oh1, oh2 = B['sm'], B['Lg'], B['goh'], B['gex'], B['selv'], B['sel'], B['sel2'], B['oh1'], B['oh2']
            A1, A2, Aa, rank, valid, slot, tmp32, h2 = B['A1'], B['A2'], B['Aa'], B['rank'], B['valid'], B['slot'], B['tmp32'], B['h2']
            P.op('vector', lambda e: e.tensor_reduce(out=S_['gmax'][:, :], in_=Lg[:, 0:4], axis=AX.X, op=ALU.max), reads=[Lg], writes=[S_['gmax']])
            P.ts('vector', goh[:, :], Lg[:, 0:4], S_['gmax'][:, 0:1], None, ALU.is_equal, reads=[Lg, S_['gmax']], writes=[goh])
            P.ts('vector', gex[:, :], Lg[:, 0:4], S_['gmax'][:, 0:1], None, ALU.subtract, reads=[Lg, S_['gmax']], writes=[gex])
            P.tt('gpsimd', gex[:, :], CONST['e'][:, 0:4], gex[:, :], ALU.pow, reads=[CONST['e'], gex], writes=[gex])
            P.op('vector', lambda e: e.tensor_reduce(out=S_['gsum'][:, :], in_=gex[:, :], axis=AX.X, op=ALU.add), reads=[gex], writes=[S_['gsum']])
            P.op('vector', lambda e: e.reciprocal(S_['grp_p'][:, :], S_['gsum'][:, :]), reads=[S_['gsum']], writes=[S_['grp_p']])
            P.tt('vector', v48(selv), Lg[:, 4:36].rearrange("p (g e) -> p g e", g=4), goh[:, :].unsqueeze(2).to_broadcast([128, 4, 8]),
                 ALU.mult, reads=[Lg, goh], writes=[selv])
            P.op('vector', lambda e: e.tensor_reduce(out=sel[:, :], in_=selv[:, :].rearrange("p (g e) -> p e g", g=4), axis=AX.X, op=ALU.add),
                 reads=[selv], writes=[sel])
            P.op('vector', lambda e: e.tensor_reduce(out=S_['m1'][:, :], in_=sel[:, :], axis=AX.X, op=ALU.max), reads=[sel], writes=[S_['m1']])
            P.ts('vector', oh1[:, :], sel[:, :], S_['m1'][:, 0:1], None, ALU.is_equal, reads=[sel, S_['m1']], writes=[oh1])
            P.op('vector', lambda e: e.scalar_tensor_tensor(out=sel2[:, :], in0=oh1[:, :], scalar=-1e30, in1=sel[:, :], op0=ALU.mult, op1=ALU.add),
                 reads=[oh1, sel], writes=[sel2])
            P.op('vector', lambda e: e.tensor_reduce(out=S_['m2'][:, :], in_=sel2[:, :], axis=AX.X, op=ALU.max), reads=[sel2], writes=[S_['m2']])
            P.ts('vector', oh2[:, :], sel2[:, :], S_['m2'][:, 0:1], None, ALU.is_equal, reads=[sel2, S_['m2']], writes=[oh2])
            P.tt('vector', S_['dm'][:, :], S_['m2'][:, :], S_['m1'][:, :], ALU.subtract, reads=[S_['m1'], S_['m2']], writes=[S_['dm']])
            P.tt('gpsimd', S_['e21'][:, :], CONST['e'][:, 0:1], S_['dm'][:, :], ALU.pow, reads=[CONST['e'], S_['dm']], writes=[S_['e21']])
            P.ts('vector', S_['den'][:, :], S_['e21'][:, :], 1.0, None, ALU.add, reads=[S_['e21']], writes=[S_['den']])
            P.op('vector', lambda e: e.reciprocal(S_['den'][:, :], S_['den'][:, :]), reads=[S_['den']], writes=[S_['den']])
            P.tt('vector', S_['g1w'][:, :], S_['grp_p'][:, :], S_['den'][:, :], ALU.mult, reads=[S_['grp_p'], S_['den']], writes=[S_['g1w']])
            P.tt('vector', S_['g2w'][:, :], S_['g1w'][:, :], S_['e21'][:, :], ALU.mult, reads=[S_['g1w'], S_['e21']], writes=[S_['g2w']])
            for (A_, oh_) in ((A1, oh1), (A2, oh2)):
                P.copy('vector', v48(A_), goh[:, :].unsqueeze(2).to_broadcast([128, 4, 8]), reads=[goh], writes=[A_])
                P.tt('vector', v48(A_), v48(A_), oh_[:, :].unsqueeze(1).to_broadcast([128, 4, 8]), ALU.mult, reads=[A_, oh_], writes=[A_])
            P.tt('vector', Aa[:, :], A1[:, :], A2[:, :], ALU.add, reads=[A1, A2], writes=[Aa])
            pr = ring.next()
            P.mm(pr, pr[:, 0:32], Us[:, :], Aa[:, :], reads=[Us, Aa])
            P.tt('vector', rank[:, :], pr[:, 0:32], Rrun[:, :], ALU.add, reads=[pr, Rrun], writes=[rank])
            pr2 = ring.next()
            P.mm(pr2, pr2[:, 0:32], ones[:, :], Aa[:, :], reads=[ones, Aa])
            P.tt('vector', Rrun[:, :], pr2[:, 0:32], Rrun[:, :], ALU.add, reads=[pr2, Rrun], writes=[Rrun])
            P.ts('vector', valid[:, :], rank[:, :], float(CAP), None, ALU.is_lt, reads=[rank], writes=[valid])
            P.tt('vector', slot[:, :], rank[:, :], ebase[:, :], ALU.add, reads=[rank, ebase], writes=[slot])
            P.tt('vector', slot[:, :], slot[:, :], valid[:, :], ALU.mult, reads=[slot, valid], writes=[slot])
            for (A_, if_, v_, gw) in ((A1, S_['i1f'], S_['v1'], S_['g1w']), (A2, S_['i2f'], S_['v2'], S_['g2w'])):
                P.tt('vector', tmp32[:, :], A_[:, :], slot[:, :], ALU.mult, reads=[A_, slot], writes=[tmp32])
                P.op('vector', lambda e, if_=if_: e.tensor_reduce(out=if_[:, :], in_=tmp32[:, :], axis=AX.X, op=ALU.add),
                     reads=[tmp32], writes=[if_])
                P.ts('vector', if_[:, :], if_[:, :], float(TRASH), None, ALU.add, reads=[if_], writes=[if_])
                P.tt('vector', tmp32[:, :], A_[:, :], valid[:, :], ALU.mult, reads=[A_, valid], writes=[tmp32])
                P.op('vector', lambda e, v_=v_: e.tensor_reduce(out=v_[:, :], in_=tmp32[:, :], axis=AX.X, op=ALU.add),
                     reads=[tmp32], writes=[v_])
                P.tt('vector', gw[:, :], gw[:, :], v_[:, :], ALU.mult, reads=[gw, v_], writes=[gw])
            P.copy('vector', RTi[:, i, 0:1], S_['i1f'][:, :], reads=[S_['i1f']], writes=[RTi])
            P.copy('vector', RTi[:, i, 1:2], S_['i2f'][:, :], reads=[S_['i2f']], writes=[RTi])
            P.copy('vector', RT[:, i, 0:1], S_['g1w'][:, :], reads=[S_['g1w']], writes=[RT])
            P.copy('vector', RT[:, i, 1:2], S_['g2w'][:, :], reads=[S_['g2w']], writes=[RT])

        def st_e(i, B):
            h2 = B['h2']
            for k in range(2):
                P.dma('gpsimd', [(XE[:, :], h2.ap())], reads=[h2, RTi], tile=h2,
                      indirect=dict(out_offset=bass.IndirectOffsetOnAxis(RTi[:, i, k:k + 1], 0), in_offset=None))
        tiles = [i for i in range(NTILE) if only is None or i in only]
        pairs = [tiles[g0:g0 + 2] for g0 in range(0, len(tiles), 2)]

        def run(stage, p):
            for j, i in enumerate(pairs[p]):
                stage(i, slots[(j, p % 2)])
        if pairs:
            run(st_a, 0)
            run(st_b, 0)
            run(st_c, 0)
        for p in range(len(pairs)):
            if p + 1 < len(pairs):
                run(st_a, p + 1)
                run(st_b, p + 1)
                for j in range(2):
                    if j < len(pairs[p + 1]):
                        st_c(pairs[p + 1][j], slots[(j, (p + 1) % 2)])
                    if j < len(pairs[p]):
                        st_d(pairs[p][j], slots[(j, p % 2)])
            else:
                run(st_d, p)
            run(st_e, p)


def phase7(P, io, ps, ident, experts=range(32)):
    XE, YE = io['XE'], io['YE']
    ring = PsRing(ps)
    with ExitStack() as les:
        def tl(name, shape, dt=F32):
            return P.tile("p7_" + name, list(shape), dt, es=les)
        wg = [tl("wg%d" % k, (128, 8, 512)) for k in range(2)]
        wu = [tl("wu%d" % k, (128, 8, 512)) for k in range(2)]
        wd = [tl("wd%d" % k, (128, 4, D)) for k in range(2)]
        NBUF = 2
        xb = [tl("xb%d" % k, (128, D)) for k in range(2 * NBUF)]
        xT = [tl("xT%d" % k, (128, 8, 128)) for k in range(NBUF)]
        sg = [tl("sg%d" % k, (128, 512)) for k in range(NBUF)]
        hm = [tl("hm%d" % k, (128, 512)) for k in range(NBUF)]
        hT = [tl("hT%d" % k, (128, 4, 128)) for k in range(NBUF)]
        yb = [tl("yb%d" % k, (128, D)) for k in range(NBUF)]
        wgv = io['w_gate'].rearrange("(e c p) n -> e p c n", p=128, c=8)
        wuv = io['w_up'].rearrange("(e c p) n -> e p c n", p=128, c=8)
        wdv = io['w_down'].rearrange("(e c p) n -> e p c n", p=128, c=4)
        experts = list(experts)
        nblk = CAP // 128

        def loadw(ei):
            k = ei % 2
            P.dma('sync', [(wg[k].ap(), wgv[experts[ei]])], writes=[wg[k]])
            P.dma('sync', [(wu[k].ap(), wuv[experts[ei]])], writes=[wu[k]])
            P.dma('sync', [(wd[k].ap(), wdv[experts[ei]])], writes=[wd[k]])
        blocks = [(ei, blk) for ei in range(len(experts)) for blk in range(nblk)]

        def loadx(bi):
            ei, blk = blocks[bi]
            r0 = experts[ei] * CAP + blk * 128
            P.dma('sync', [(xb[bi % (2 * NBUF)].ap(), XE[r0:r0 + 128, :])], writes=[xb[bi % (2 * NBUF)]])
        loadw(0)
        for bi in range(min(2 * NBUF, len(blocks))):
            loadx(bi)
        st = {}

        def s_tr(bi, j):
            x_ = xb[bi % (2 * NBUF)]
            for hf in range(2):
                pq = ring.next()
                for q in range(4):
                    c = hf * 4 + q
                    P.transpose(pq, pq[:, q * 128:(q + 1) * 128], x_[:, c * 128:(c + 1) * 128], ident, reads=[x_])
                P.copy('scalar' if hf else 'vector', xT[j][:, hf * 4:hf * 4 + 4, :], pq[:, :].rearrange("p (c t) -> p c t", c=4),
                       reads=[pq], writes=[xT[j]])

        def s_gu(bi, j):
            k = blocks[bi][0] % 2
            pg, pu = ring.next(), ring.next()
            for c in range(8):
                P.mm(pg, pg[:, :], xT[j][:, c, :], wg[k][:, c, :], start=(c == 0), stop=(c == 7), reads=[xT[j], wg[k]])
            for c in range(8):
                P.mm(pu, pu[:, :], xT[j][:, c, :], wu[k][:, c, :], start=(c == 0), stop=(c == 7), reads=[xT[j], wu[k]])
            P.act(sg[j][:, :], pg[:, :], AF.Silu, reads=[pg], writes=[sg[j]])
            P.tt('vector', hm[j][:, :], pu[:, :], sg[j][:, :], ALU.mult, reads=[pu, sg[j]], writes=[hm[j]])
            if bi + 2 * NBUF < len(blocks):
                loadx(bi + 2 * NBUF)

        def s_t4(bi, j):
            pq = ring.next()
            for c in range(4):
                P.transpose(pq, pq[:, c * 128:(c + 1) * 128], hm[j][:, c * 128:(c + 1) * 128], ident, reads=[hm[j]])
            P.copy('scalar', hT[j][:, :, :], pq[:, :].rearrange("p (c t) -> p c t", c=4), reads=[pq], writes=[hT[j]])

        def s_dn(bi, j):
            ei, blk = blocks[bi]
            k = ei % 2
            r0 = experts[ei] * CAP + blk * 128
            for hf in range(2):
                pq = ring.next()
                for c in range(4):
                    P.mm(pq, pq[:, :], hT[j][:, c, :], wd[k][:, c, hf * 512:(hf + 1) * 512], start=(c == 0), stop=(c == 3),
                         reads=[hT[j], wd[k]])
                P.copy('vector' if hf else 'scalar', yb[j][:, hf * 512:(hf + 1) * 512], pq[:, :], reads=[pq], writes=[yb[j]])
            P.dma('sync', [(YE[r0:r0 + 128, :], yb[j].ap())], reads=[yb[j]])
        loaded = 1
        for g0 in range(0, len(blocks), NBUF):
            grp = list(range(g0, min(g0 + NBUF, len(blocks))))
            lo, hi = min(blocks[bi][0] for bi in grp), max(blocks[bi][0] for bi in grp)
            while loaded < hi + 1:
                loadw(loaded)
                loaded += 1
            if lo == hi and loaded == hi + 1 and hi + 1 < len(experts):
                loadw(hi + 1)
                loaded += 1
            for stage in (s_tr, s_gu, s_t4, s_dn):
                for j, bi in enumerate(grp):
                    stage(bi, j)


def phase8(P, io, RT, RTi, only=None):
    X1, YE, y = io['X1'], io['YE'], io['y']
    with ExitStack() as les:
        def tl(name, shape, dt=F32):
            return P.tile("p8_" + name, list(shape), dt, es=les)
        x1 = [tl("x1_%d" % k, (128, D)) for k in range(2)]
        ya = [tl("ya_%d" % k, (128, D)) for k in range(2)]
        yb = [tl("yb_%d" % k, (128, D)) for k in range(2)]
        for i in range(NTILE):
            if only is not None and i not in only:
                continue
            n0 = i * 128
            k = i % 2
            P.dma('sync', [(x1[k].ap(), X1[n0:n0 + 128, :])], writes=[x1[k]])
            P.dma('gpsimd', [(ya[k].ap(), YE[:, :])], reads=[RTi], writes=[ya[k]],
                  indirect=dict(out_offset=None, in_offset=bass.IndirectOffsetOnAxis(RTi[:, i, 0:1], 0)))
            P.dma('gpsimd', [(yb[k].ap(), YE[:, :])], reads=[RTi], writes=[yb[k]],
                  indirect=dict(out_offset=None, in_offset=bass.IndirectOffsetOnAxis(RTi[:, i, 1:2], 0)))
            P.op('vector', lambda e: e.scalar_tensor_tensor(out=x1[k][:, :], in0=ya[k][:, :], scalar=RT[:, i, 0:1], in1=x1[k][:, :],
                                                            op0=ALU.mult, op1=ALU.add), reads=[ya[k], RT, x1[k]], writes=[x1[k]])
            P.op('vector', lambda e: e.scalar_tensor_tensor(out=x1[k][:, :], in0=yb[k][:, :], scalar=RT[:, i, 1:2], in1=x1[k][:, :],
                                                            op0=ALU.mult, op1=ALU.add), reads=[yb[k], RT, x1[k]], writes=[x1[k]])
            P.dma('sync', [(y[n0:n0 + 128, :], x1[k].ap())], reads=[x1[k]])


_NC_CACHE = {}


def kernel(**inputs):
    inputs = {k: np.asarray(v) for k, v in inputs.items()}
    if 'nc' not in _NC_CACHE:
        _NC_CACHE['nc'] = build_program([1, 2, 35, 45, 6, 7, 8])
    nc = _NC_CACHE['nc']
    n_cores = 8
    in_maps = [make_inputs(inputs, c) for c in range(n_cores)]
    res = run_bass_kernel_spmd(nc, in_maps, core_ids=list(range(n_cores)))
    out = np.stack([np.asarray(res.results[c]['y']).reshape(NB, T, D) for c in range(n_cores)], axis=0)
    return out.reshape(16, T, D).astype(np.float32)
```
